# Optimizing a Trainium2 kernel written in Bass

```python
import math
import jax, jax.numpy as jnp
from jax import lax
import numpy as np


D_MODEL = 2048
BATCH = 8
SEQ = 2048
DEPTH = 4

GRID_W = 64
CTX_LEN = 256
MIX_W = D_MODEL
GROUP_W = MIX_W // 4
POOL_GROUPS = 4
POOL_GROUP_DIM = GROUP_W // POOL_GROUPS
POOL_WINDOWS = (2, 4, 8, 16)
GLA_HEADS = 4
GLA_HEAD_DIM = GROUP_W // GLA_HEADS
GLA_GATE_RANK = 16
GLA_GATE_NORM = 16.0
GLA_CHUNK = 64
CONV_W = GROUP_W
CONV_K = 31
DIFF_HEADS = 4
DIFF_QK_DIM = GROUP_W // (2 * DIFF_HEADS)
DIFF_V_DIM = GROUP_W // DIFF_HEADS
ROPE_BASE = 10000.0
ROPE_AXIS_DIM = DIFF_QK_DIM // 2
Q_BLOCK = 128
N_EXPERTS = 16
EXPERT_FF = D_MODEL // 2
EC_CAPACITY = 2
NORM_EPS = 1e-6
IN_SIZES = (GROUP_W, GROUP_W, GROUP_W, GROUP_W, GROUP_W, GLA_GATE_RANK, GLA_GATE_RANK, 2 * CONV_W, GROUP_W, GROUP_W, GROUP_W)
IN_W = 8 * GROUP_W + 2 * GLA_GATE_RANK + 2 * CONV_W

kernel_name = 'hybrid_pool_gla_conformer_diffattn_ec_moe_dit'


def rms_norm(x, g):
    xf = x.astype(jnp.float32)
    y = xf * lax.rsqrt(jnp.mean(xf * xf, axis=-1, keepdims=True) + NORM_EPS)
    return (y * g.astype(jnp.float32)).astype(x.dtype)


def layer_norm(x, g, b):
    xf = x.astype(jnp.float32)
    mu = jnp.mean(xf, axis=-1, keepdims=True)
    var = jnp.mean(jnp.square(xf - mu), axis=-1, keepdims=True)
    y = (xf - mu) * lax.rsqrt(var + NORM_EPS) * g.astype(jnp.float32) + b.astype(jnp.float32)
    return y.astype(x.dtype)


def modulate(h, shift, scale):
    return h * (1.0 + scale) + shift


def split_cols(u):
    parts, start = [], 0
    for size in IN_SIZES:
        parts.append(u[..., start:start + size])
        start += size
    return parts


def multi_scale_pool(u, w_pool, scale):
    B, T, _ = u.shape
    ug = u.astype(jnp.float32).reshape(B, T, POOL_GROUPS, POOL_GROUP_DIM)
    cs = jnp.pad(jnp.cumsum(ug, axis=1), ((0, 0), (1, 0), (0, 0), (0, 0)))
    t = jnp.arange(T)
    outs = []
    for gi, w in enumerate(POOL_WINDOWS):
        lo = jnp.clip(t - w // 2, 0, T)
        hi = jnp.clip(t + w // 2, 0, T)
        mean = (cs[:, hi, gi] - cs[:, lo, gi]) / (hi - lo).astype(jnp.float32)[:, None]
        outs.append(mean - ug[:, :, gi])
    p = jnp.stack(outs, axis=2)
    y = jnp.einsum('btgc,gcd->btgd', p, w_pool.astype(jnp.float32)).reshape(B, T, GROUP_W)
    return (y * scale.astype(jnp.float32)).astype(u.dtype)


def gla_heads(a):
    B, T, _ = a.shape
    return a.reshape(B, T, GLA_HEADS, -1).transpose(0, 2, 1, 3).astype(jnp.float32)


def gla_inputs(q, k, v, low_f, low_b, up_f, bias_f, up_b, bias_b):
    g_f = jax.nn.log_sigmoid((low_f @ up_f + bias_f).astype(jnp.float32)) / GLA_GATE_NORM
    g_b = jax.nn.log_sigmoid((low_b @ up_b + bias_b).astype(jnp.float32)) / GLA_GATE_NORM
    return (gla_heads(q) * GLA_HEAD_DIM ** -0.5, gla_heads(k), gla_heads(v), gla_heads(g_f), gla_heads(g_b))


def gla_chunk_scan(q, k, v, g, s0):
    B, H, T, _ = q.shape
    n = T // GLA_CHUNK

    def chunks(a):
        return jnp.moveaxis(a.reshape(B, H, n, GLA_CHUNK, a.shape[-1]), 2, 0)

    mask = jnp.tril(jnp.ones((GLA_CHUNK, GLA_CHUNK), dtype=bool))[:, :, None]

    def step(s, inp):
        qc, kc, vc, gc = inp
        G = jnp.cumsum(gc, axis=2)
        G_last = G[:, :, -1:, :]
        o_inter = jnp.einsum('bhik,bhkv->bhiv', qc * jnp.exp(G), s)
        decay = jnp.exp(jnp.where(mask, G[:, :, :, None, :] - G[:, :, None, :, :], -jnp.inf))
        att = jnp.einsum('bhik,bhjk,bhijk->bhij', qc, kc, decay)
        o_intra = jnp.einsum('bhij,bhjv->bhiv', att, vc)
        s_new = jnp.exp(G_last[:, :, 0, :, None]) * s + jnp.einsum('bhjk,bhjv->bhkv', kc * jnp.exp(G_last - G), vc)
        return s_new, o_inter + o_intra

    s_fin, o = lax.scan(step, s0, (chunks(q), chunks(k), chunks(v), chunks(g)))
    return jnp.moveaxis(o, 0, 2).reshape(B, H, T, -1), s_fin


def gla_bidir(q, k, v, g_f, g_b, s0_f, s0_b):
    flip = lambda a: jnp.flip(a, axis=2)
    o_f, s_f = gla_chunk_scan(q, k, v, g_f, s0_f)
    o_b, s_b = gla_chunk_scan(flip(q), flip(k), flip(v), flip(g_b), s0_b)
    return o_f + flip(o_b), s_f, s_b


def gla_out(o, gate, norm_g):
    B, H, T, dv = o.shape
    o = rms_norm(o.transpose(0, 2, 1, 3), norm_g).reshape(B, T, H * dv)
    return (o * jax.nn.silu(gate.astype(jnp.float32))).astype(gate.dtype)


def conformer_conv(u, dw, dw_b, ln_g, ln_b, pw, pw_b):
    a, gt = jnp.split(u, 2, axis=-1)
    h = a * jax.nn.sigmoid(gt)
    h = lax.conv_general_dilated(h, dw[:, None, :].astype(h.dtype), window_strides=(1,),
                                 padding=[(CONV_K // 2, CONV_K // 2)],
                                 dimension_numbers=('NWC', 'WIO', 'NWC'),
                                 feature_group_count=CONV_W) + dw_b
    h = layer_norm(h, ln_g, ln_b)
    return jax.nn.silu(h) @ pw + pw_b


def rotate(x, cos, sin):
    x1, x2 = jnp.split(x, 2, axis=-1)
    return jnp.concatenate([x1 * cos - x2 * sin, x2 * cos + x1 * sin], axis=-1)


def rope_2d(x, rope):
    cos_r, sin_r, cos_c, sin_c = rope
    xr, xc = jnp.split(x, 2, axis=-1)
    return jnp.concatenate([rotate(xr, cos_r, sin_r), rotate(xc, cos_c, sin_c)], axis=-1)


def diff_split_qk(a):
    B, T, _ = a.shape
    a = a.reshape(B, T, DIFF_HEADS, 2, DIFF_QK_DIM).transpose(3, 0, 2, 1, 4)
    return a[0], a[1]


def diff_split_v(a):
    B, T, _ = a.shape
    return a.reshape(B, T, DIFF_HEADS, DIFF_V_DIM).transpose(0, 2, 1, 3)


def diff_maps(q1, q2, k1, k2, v, lam):
    scale = DIFF_QK_DIM ** -0.5
    s1 = jnp.einsum('bhqd,bhkd->bhqk', q1, k1).astype(jnp.float32) * scale
    s2 = jnp.einsum('bhqd,bhkd->bhqk', q2, k2).astype(jnp.float32) * scale
    p = jax.nn.softmax(s1, axis=-1) - lam * jax.nn.softmax(s2, axis=-1)
    return jnp.einsum('bhqk,bhkv->bhqv', p.astype(v.dtype), v)


def diff_latent(q1, q2, k1, k2, v, lam):
    B, H, N, _ = q1.shape
    nb = N // Q_BLOCK
    blk = lambda a: jnp.moveaxis(a.reshape(B, H, nb, Q_BLOCK, a.shape[-1]), 2, 0)
    o = lax.map(lambda qs: diff_maps(qs[0], qs[1], k1, k2, v, lam), (blk(q1), blk(q2)))
    return jnp.moveaxis(o, 0, 2).reshape(B, H, N, -1)


def diff_finish(o, subln_g, lam_init):
    B, H, T, dv = o.shape
    o = rms_norm(o, subln_g) * (1.0 - lam_init)
    return o.transpose(0, 2, 1, 3).reshape(B, T, H * dv)


def expert_choice_ffn(h, w_router, w_gate, w_up, w_down):
    B, n, _ = h.shape
    cap = EC_CAPACITY * n // N_EXPERTS
    aff = jax.nn.softmax((h @ w_router).astype(jnp.float32), axis=-1)
    gate, idx = lax.top_k(jnp.swapaxes(aff, 1, 2), cap)
    bidx = jnp.arange(B)[:, None, None]
    xe = h[bidx, idx]
    a = jnp.einsum('becd,edf->becf', xe, w_gate)
    u = jnp.einsum('becd,edf->becf', xe, w_up)
    y = jnp.einsum('becf,efd->becd', jax.nn.silu(a) * u, w_down) * gate[..., None].astype(h.dtype)
    return jnp.zeros_like(h).at[bidx, idx].add(y)


def setup_inputs(seed: int = 0) -> dict:
    key = jax.random.key(seed)
    ks = iter(jax.random.split(key, 40))
    nrm = lambda shape, s: jax.random.normal(next(ks), shape, jnp.float32) * s
    gain = lambda shape: 1.0 + nrm(shape, 0.02)
    L, D, F, E = DEPTH, D_MODEL, EXPERT_FF, N_EXPERTS
    return {
        'x': nrm((BATCH, SEQ, D), 1.0),
        'c': nrm((BATCH, D), 1.0),
        'ctx': nrm((BATCH, CTX_LEN, D), 1.0),
        'c_ctx': nrm((D,), 1.0),
        'w_ada': nrm((L, D, 6 * D), 0.5 * D ** -0.5),
        'b_ada': nrm((L, 6 * D), 0.02),
        'norm1_g': gain((L, D)),
        'norm2_g': gain((L, D)),
        'w_in': nrm((L, D, IN_W), D ** -0.5),
        'pool_w': nrm((L, POOL_GROUPS, POOL_GROUP_DIM, POOL_GROUP_DIM), POOL_GROUP_DIM ** -0.5),
        'pool_scale': gain((L, GROUP_W)),
        'gla_gk_up_f': nrm((L, GLA_GATE_RANK, GROUP_W), GLA_GATE_RANK ** -0.5),
        'gla_gk_bias_f': nrm((L, GROUP_W), 0.02),
        'gla_gk_up_b': nrm((L, GLA_GATE_RANK, GROUP_W), GLA_GATE_RANK ** -0.5),
        'gla_gk_bias_b': nrm((L, GROUP_W), 0.02),
        'gla_norm_g': gain((L, GLA_HEAD_DIM)),
        'conv_dw': nrm((L, CONV_K, CONV_W), CONV_K ** -0.5),
        'conv_dw_b': nrm((L, CONV_W), 0.02),
        'conv_ln_g': gain((L, CONV_W)),
        'conv_ln_b': nrm((L, CONV_W), 0.02),
        'conv_pw': nrm((L, CONV_W, CONV_W), CONV_W ** -0.5),
        'conv_pw_b': nrm((L, CONV_W), 0.02),
        'diff_lq1': nrm((L, DIFF_QK_DIM), 0.1),
        'diff_lk1': nrm((L, DIFF_QK_DIM), 0.1),
        'diff_lq2': nrm((L, DIFF_QK_DIM), 0.1),
        'diff_lk2': nrm((L, DIFF_QK_DIM), 0.1),
        'diff_subln_g': gain((L, DIFF_V_DIM)),
        'w_out': nrm((L, MIX_W, D), MIX_W ** -0.5),
        'w_router': nrm((L, D, E), D ** -0.5),
        'w_exp_gate': nrm((L, E, D, F), D ** -0.5),
        'w_exp_up': nrm((L, E, D, F), D ** -0.5),
        'w_exp_down': nrm((L, E, F, D), F ** -0.5),
        'final_norm_g': gain((D,)),
    }


def reference(x, c, ctx, c_ctx, w_ada, b_ada, norm1_g, norm2_g, w_in, pool_w, pool_scale,
              gla_gk_up_f, gla_gk_bias_f, gla_gk_up_b, gla_gk_bias_b, gla_norm_g,
              conv_dw, conv_dw_b, conv_ln_g, conv_ln_b, conv_pw, conv_pw_b,
              diff_lq1, diff_lk1, diff_lq2, diff_lk2, diff_subln_g,
              w_out, w_router, w_exp_gate, w_exp_up, w_exp_down, final_norm_g):
    B, N, D = x.shape
    ROWS = N // GRID_W
    row = jnp.repeat(jnp.arange(ROWS), GRID_W).astype(jnp.float32)
    col = jnp.tile(jnp.arange(GRID_W), ROWS).astype(jnp.float32)
    inv_freq = ROPE_BASE ** (-jnp.arange(0, ROPE_AXIS_DIM, 2, dtype=jnp.float32) / ROPE_AXIS_DIM)
    ang_r = row[:, None] * inv_freq
    ang_c = col[:, None] * inv_freq
    rope = (jnp.cos(ang_r).astype(x.dtype), jnp.sin(ang_r).astype(x.dtype),
            jnp.cos(ang_c).astype(x.dtype), jnp.sin(ang_c).astype(x.dtype))

    sc = jax.nn.silu(c)
    sc_ctx = jax.nn.silu(c_ctx)
    for l in range(DEPTH):
        last = l == DEPTH - 1
        mod_l = jnp.split((sc @ w_ada[l] + b_ada[l])[:, None, :], 6, axis=-1)
        mod_c = jnp.split(sc_ctx @ w_ada[l] + b_ada[l], 6, axis=-1)
        h_l = modulate(rms_norm(x, norm1_g[l]), mod_l[0], mod_l[1])
        h_c = modulate(rms_norm(ctx, norm1_g[l]), mod_c[0], mod_c[1])
        p_l = split_cols(h_l @ w_in[l])
        p_c = split_cols(h_c @ w_in[l])

        pool_l = multi_scale_pool(p_l[0], pool_w[l], pool_scale[l])

        gp = (gla_gk_up_f[l], gla_gk_bias_f[l], gla_gk_up_b[l], gla_gk_bias_b[l])
        gq_c, gk_c, gv_c, gf_c, gb_c = gla_inputs(p_c[1], p_c[2], p_c[3], p_c[5], p_c[6], *gp)
        gq_l, gk_l, gv_l, gf_l, gb_l = gla_inputs(p_l[1], p_l[2], p_l[3], p_l[5], p_l[6], *gp)
        s0 = jnp.zeros((B, GLA_HEADS, GLA_HEAD_DIM, GLA_HEAD_DIM), jnp.float32)
        go_c, s_f, s_b = gla_bidir(gq_c, gk_c, gv_c, gf_c, gb_c, s0, s0)
        go_l, _, _ = gla_bidir(gq_l, gk_l, gv_l, gf_l, gb_l, s_f, s_b)
        gla_l = gla_out(go_l, p_l[4], gla_norm_g[l])

        cp = (conv_dw[l], conv_dw_b[l], conv_ln_g[l], conv_ln_b[l], conv_pw[l], conv_pw_b[l])
        conv_l = conformer_conv(p_l[7], *cp)

        lam_init = 0.8 - 0.6 * math.exp(-0.3 * l)
        lam = (jnp.exp(jnp.sum(diff_lq1[l] * diff_lk1[l]).astype(jnp.float32))
               - jnp.exp(jnp.sum(diff_lq2[l] * diff_lk2[l]).astype(jnp.float32)) + lam_init)
        q1_c, q2_c = diff_split_qk(p_c[8])
        k1_c, k2_c = diff_split_qk(p_c[9])
        dv_c = diff_split_v(p_c[10])
        q1_l, q2_l = diff_split_qk(p_l[8])
        k1_l, k2_l = diff_split_qk(p_l[9])
        dv_l = diff_split_v(p_l[10])
        k1_all = jnp.concatenate([rope_2d(k1_l, rope), k1_c], axis=2)
        k2_all = jnp.concatenate([rope_2d(k2_l, rope), k2_c], axis=2)
        v_all = jnp.concatenate([dv_l, dv_c], axis=2)
        d_o = diff_latent(rope_2d(q1_l, rope), rope_2d(q2_l, rope), k1_all, k2_all, v_all, lam)
        diff_l = diff_finish(d_o, diff_subln_g[l], lam_init)

        x = x + mod_l[2] * (jnp.concatenate([pool_l, gla_l, conv_l, diff_l], axis=-1) @ w_out[l])
        h2_l = modulate(rms_norm(x, norm2_g[l]), mod_l[3], mod_l[4])
        x = x + mod_l[5] * expert_choice_ffn(h2_l, w_router[l], w_exp_gate[l], w_exp_up[l], w_exp_down[l])

        if not last:
            pool_c = multi_scale_pool(p_c[0], pool_w[l], pool_scale[l])
            gla_c = gla_out(go_c, p_c[4], gla_norm_g[l])
            conv_c = conformer_conv(p_c[7], *cp)
            diff_c = diff_finish(diff_maps(q1_c, q2_c, k1_c, k2_c, dv_c, lam), diff_subln_g[l], lam_init)
            ctx = ctx + mod_c[2] * (jnp.concatenate([pool_c, gla_c, conv_c, diff_c], axis=-1) @ w_out[l])
            h2_c = modulate(rms_norm(ctx, norm2_g[l]), mod_c[3], mod_c[4])
            ctx = ctx + mod_c[5] * expert_choice_ffn(h2_c, w_router[l], w_exp_gate[l], w_exp_up[l], w_exp_down[l])

    return rms_norm(x, final_norm_g)
```

```python
import math
from contextlib import ExitStack
import numpy as np
import concourse.bass as bass
import concourse.mybir as mybir
from concourse.bass_utils import run_bass_kernel_spmd

F32 = mybir.dt.float32
BF16 = mybir.dt.bfloat16
I32 = mybir.dt.int32
U32 = mybir.dt.uint32
AF = mybir.ActivationFunctionType
ALU = mybir.AluOpType
AX = mybir.AxisListType

D = 2048
T_L = 2048
T_C = 256
NT = T_L + T_C
NTILE = NT // 128
DEPTH = 4
IN_W = 5152
NE = 16
FF = 1024
CAP_L = 256
CAP_C = 32
EPS = 1e-6
NCOL = 512

C_N1G, C_N2G = 0, 16
C_PSC, C_GBF, C_GBB = 32, 36, 40
C_GNG, C_CDB, C_CLG, C_CLB, C_CPB = 44, 45, 49, 53, 57
C_SLG, C_LAMI, C_OML = 61, 62, 63
C_DW = 64
C_LQ = 192
K_ID, K_ONE, K_PM, K_UF, K_UB, K_MF, K_MB = 0, 128, 256, 384, 512, 640, 768
NKF = 896

PF_POOL, PF_GQ, PF_GK, PF_GG, PF_LOW, PF_CA, PF_CG, PF_DQ, PF_DK = 0, 512, 1024, 1536, 2048, 2080, 2592, 3104, 3616
PF_ROWS = 4128
TM_GK, TM_GV, TM_DV = 0, 512, 1024


class KB:
    def __init__(self, nc, stack):
        self.nc = nc
        self.E = {'pe': nc.tensor, 'act': nc.scalar, 'dve': nc.vector, 'pool': nc.gpsimd, 'sp': nc.sync}
        self.sem = {}
        self.cnt = {}
        for e in self.E:
            self.sem[e] = stack.enter_context(nc.semaphore("s_" + e))
            self.cnt[e] = 0
        self.NR = 16
        self.dsem = {q: [stack.enter_context(nc.semaphore(f"d_{q}{i}")) for i in range(self.NR)] for q in ('sp', 'pool')}
        self.dcnt = {q: 0 for q in ('sp', 'pool')}
        self.waited = {e: {} for e in self.E}
        self.res = {}
        self.n_ins = 0

    def semof(self, k):
        if isinstance(k, tuple):
            return self.dsem[k[0]][k[1]]
        return self.sem[k]

    def _wait(self, e, tok):
        if tok is None:
            return
        k, v = tok
        if k == 'pe' and e == 'pe':
            return
        if self.waited[e].get(k, 0) >= v:
            return
        self.E[e].wait_ge(self.semof(k), v)
        self.waited[e][k] = v
        self.n_ins += 1

    def _deps(self, reads, writes):
        d = []
        for k in reads:
            r = self.res.get(k)
            if r and r[0]:
                d.append(r[0])
        for k in writes:
            r = self.res.get(k)
            if r:
                if r[0]:
                    d.append(r[0])
                d.extend(r[1].items())
        return d

    def _commit(self, tok, reads, writes):
        for k in reads:
            r = self.res.setdefault(k, [None, {}])
            if r[1].get(tok[0], 0) < tok[1]:
                r[1][tok[0]] = tok[1]
        for k in writes:
            self.res[k] = [tok, {}]

    def op(self, e, fn, reads=(), writes=()):
        for t in self._deps(reads, writes):
            self._wait(e, t)
        ins = fn(self.E[e])
        self.cnt[e] += 1
        ins.then_inc(self.sem[e], 1)
        tok = (e, self.cnt[e])
        self._commit(tok, reads, writes)
        self.n_ins += 1
        return tok

    def dma(self, q, out, in_, reads=(), writes=(), indirect=None, deps=(), **kw):
        i = self.dcnt[q]
        slot = i % self.NR
        val = 16 * (i // self.NR + 1)
        if i >= self.NR:
            self._wait(q, ((q, slot), val - 16))
        for t in list(self._deps(reads, writes)) + list(deps):
            self._wait(q, t)
        if indirect is not None:
            ins = self.E[q].indirect_dma_start(out=out, in_=in_, **indirect)
        else:
            ins = self.E[q].dma_start(out=out, in_=in_, **kw)
        ins.then_inc(self.dsem[q][slot], 16)
        self.dcnt[q] += 1
        tok = ((q, slot), val)
        self._commit(tok, reads, writes)
        self.n_ins += 1
        return tok

    def barrier(self):
        toks = [(e, self.cnt[e]) for e in self.E if self.cnt[e] > 0]
        for q in self.dcnt:
            n = self.dcnt[q]
            for slot in range(min(n, self.NR)):
                last_i = ((n - 1 - slot) // self.NR) * self.NR + slot
                toks.append(((q, slot), 16 * (last_i // self.NR + 1)))
        for e in self.E:
            for t in toks:
                if t[0] == e == 'pe':
                    continue
                self._wait(e, t)
        self.res.clear()


_UID = [0]


def sb(st, nc, name, shape, dt):
    _UID[0] += 1
    return st.enter_context(nc.sbuf_tensor(f"{name}_u{_UID[0]}", list(shape), dt))


def pst(st, nc, name, shape, dt):
    return st.enter_context(nc.psum_tensor(name, list(shape), dt))


class Prog:
    def __init__(self, nc, layers, with_final, debug=False, phases=None):
        self.nc = nc
        self.layers = layers
        self.with_final = with_final
        self.debug = debug
        self.phases = phases
        L = len(layers)
        self.L = L
        dk = "ExternalOutput" if debug else "Internal"
        dt = nc.dram_tensor
        self.xs_in = dt("xs_in", [NT, D], F32, kind="ExternalInput").ap()
        self.cT = dt("cT", [128, 16, 2], F32, kind="ExternalInput").ap()
        self.cf = dt("cf", [128, NKF], F32, kind="ExternalInput").ap()
        self.ropeC = dt("ropeC", [128, T_L], F32, kind="ExternalInput").ap()
        self.ropeS = dt("ropeS", [128, T_L], F32, kind="ExternalInput").ap()
        self.segm = dt("segm", [128, NT], F32, kind="ExternalInput").ap()
        self.invc = dt("invc", [4, NT], F32, kind="ExternalInput").ap()
        self.cols = dt("cols", [L, 128, NCOL], F32, kind="ExternalInput").ap()
        self.b_ada = dt("b_ada", [L, 2, 6 * D], F32, kind="ExternalInput").ap()
        self.w_ada = dt("w_ada", [L, D, 6 * D], F32, kind="ExternalInput").ap()
        self.w_in = dt("w_in", [L, D, IN_W], F32, kind="ExternalInput").ap()
        self.pool_w = dt("pool_w", [L, 4, 128, 128], F32, kind="ExternalInput").ap()
        self.up_f = dt("up_f", [L, 17, 512], F32, kind="ExternalInput").ap()
        self.up_b = dt("up_b", [L, 17, 512], F32, kind="ExternalInput").ap()
        self.conv_pw = dt("conv_pw", [L, 512, 512], F32, kind="ExternalInput").ap()
        self.w_out = dt("w_out", [L, D, D], F32, kind="ExternalInput").ap()
        self.w_router = dt("w_router", [L, D, NE], F32, kind="ExternalInput").ap()
        self.w_eg = dt("w_eg", [L, NE, D, FF], F32, kind="ExternalInput").ap()
        self.w_eu = dt("w_eu", [L, NE, D, FF], F32, kind="ExternalInput").ap()
        self.w_ed = dt("w_ed", [L, NE, FF, D], F32, kind="ExternalInput").ap()
        self.fng = dt("fng", [1, D], F32, kind="ExternalInput").ap()
        if with_final:
            self.out = dt("out", [T_L, D], F32, kind="ExternalOutput").ap()
            self.XS = dt("XS", [NT, D], F32, kind=dk).ap()
        else:
            self.XS = dt("XS", [NT, D], F32, kind="ExternalOutput").ap()
        self.MODS = dt("MODS", [L, 2, 6 * D], F32, kind=dk).ap()
        self.PF = dt("PF", [PF_ROWS, NT], F32, kind=dk).ap()
        self.PTM = dt("PTM", [NT, 1536], BF16, kind=dk).ap()
        self.MT = dt("MT", [D, NT], BF16, kind=dk).ap()
        self.XN2 = dt("XN2", [NT, D], BF16, kind=dk).ap()
        self.AFF = dt("AFF", [NE, NT], F32, kind=dk).ap()

    def on(self, name):
        return self.phases is None or name in self.phases

    def build(self):
        nc = self.nc
        with ExitStack() as st:
            k = KB(nc, st)
            self.k = k
            self.marks = []
            self.cfs = sb(st, nc, "cfs", [128, NKF], F32)
            self.idb = sb(st, nc, "idb", [128, 128], BF16)
            self.oneb = sb(st, nc, "oneb", [128, 128], BF16)
            self.colsb = sb(st, nc, "colsb", [128, NCOL], F32)
            self.ps = [pst(st, nc, f"ps{i}", [128, 512], F32) for i in range(8)]
            k.dma('sp', self.cfs[:], self.cf, writes=['cfs'])
            k.op('dve', lambda e: e.tensor_copy(out=self.idb[:], in_=self.cfs[:, K_ID:K_ID + 128]), reads=['cfs'], writes=['idb'])
            k.op('dve', lambda e: e.tensor_copy(out=self.oneb[:], in_=self.cfs[:, K_ONE:K_ONE + 128]), reads=['cfs'], writes=['oneb'])
            for ti in range(NTILE):
                k.dma('sp', self.XS[ti * 128:(ti + 1) * 128, :], self.xs_in[ti * 128:(ti + 1) * 128, :], writes=[('XS0', ti)])
            k.barrier()
            if self.on('mods'):
                self.marks.append(('mods', 0, dict(k.cnt)))
                self.phase_mods(0)
                k.barrier()
            for li in range(self.L):
                k.dma('sp', self.colsb[:], self.cols[li], writes=['colsb'])
                k.barrier()
                if self.on('p1'):
                    with ExitStack() as st2:
                        hT = sb(st2, nc, "hT", [128, 16, NT], BF16)
                        self.marks.append(('norm1', li, dict(k.cnt)))
                        self.phase_norm1(li, hT)
                        k.barrier()
                        self.marks.append(('win', li, dict(k.cnt)))
                        self.phase_win(li, hT)
                        k.barrier()
                if self.on('pool'):
                    self.marks.append(('pool', li, dict(k.cnt)))
                    self.phase_pool(li)
                    k.barrier()
                if self.on('conv'):
                    self.marks.append(('conv', li, dict(k.cnt)))
                    self.phase_conv(li)
                    k.barrier()
                if self.on('gla'):
                    self.marks.append(('gla', li, dict(k.cnt)))
                    self.phase_gla(li)
                    k.barrier()
                if self.on('attn'):
                    self.marks.append(('attn', li, dict(k.cnt)))
                    self.phase_attn(li)
                    k.barrier()
                if self.on('wout'):
                    self.marks.append(('wout', li, dict(k.cnt)))
                    self.phase_wout(li)
                    k.barrier()
                if self.on('norm2'):
                    self.marks.append(('norm2', li, dict(k.cnt)))
                    self.phase_norm2(li)
                    k.barrier()
                if self.on('moe'):
                    self.marks.append(('moe', li, dict(k.cnt)))
                    self.phase_moe(li)
                    k.barrier()
            self.marks.append(('final', 0, dict(k.cnt)))
            if self.with_final:
                self.phase_final()
            k.barrier()
        return nc

    def phase_mods(self, li, st=None):
        nc, k = self.nc, self.k
        own = st is None
        if own:
            st = ExitStack()
        try:
            wa = [sb(st, nc, f"wa{i}", [128, 16, 512], F32) for i in range(2)]
            scT = sb(st, nc, "scT", [128, 16, 2], F32)
            brow = [sb(st, nc, f"brow{i}", [2, 512], F32) for i in range(2)]
            mrow = [sb(st, nc, f"mrow{i}", [2, 512], F32) for i in range(2)]
            k.dma('sp', scT[:], self.cT, writes=['scT'])
            k.op('act', lambda e: e.activation(out=scT[:], in_=scT[:], func=AF.Silu), reads=['scT'], writes=['scT'])
            wv = self.w_ada[li].rearrange("(kc p) n -> p kc n", p=128)
            for n in range(24):
                buf = wa[n % 2]
                bk = f"wa{n % 2}"
                k.dma('sp', buf[:], wv[:, :, n * 512:(n + 1) * 512], writes=[bk])
                k.dma('sp', brow[n % 2][:], self.b_ada[li][:, n * 512:(n + 1) * 512], writes=[f"brow{n % 2}"])
                pb = self.ps[2 + n % 2]
                pk = f"ps{2 + n % 2}"
                for kc in range(16):
                    k.op('pe', lambda e, kc=kc: e.matmul(pb[0:2, :], lhsT=scT[:, kc, :], rhs=buf[:, kc, :], start=(kc == 0), stop=(kc == 15)),
                         reads=['scT', bk], writes=[pk])
                k.op('act', lambda e: e.activation(out=mrow[n % 2][:], in_=pb[0:2, :], func=AF.Copy), reads=[pk], writes=[f"mrow{n % 2}"])
                k.op('pool', lambda e: e.tensor_tensor(out=mrow[n % 2][:], in0=mrow[n % 2][:], in1=brow[n % 2][:], op=ALU.add),
                     reads=[f"mrow{n % 2}", f"brow{n % 2}"], writes=[f"mrow{n % 2}"])
                k.dma('sp', self.MODS[li][:, n * 512:(n + 1) * 512], mrow[n % 2][:], reads=[f"mrow{n % 2}"], writes=['MODS'])
        finally:
            if own:
                st.close()

    def load_mod_cols(self, st, li, idxs, name):
        nc, k = self.nc, self.k
        t = sb(st, nc, name, [128, len(idxs), 2, 16], F32)
        for a, m in enumerate(idxs):
            for s in range(2):
                src = self.MODS[li, s, m * D:(m + 1) * D].rearrange("(kc p) -> p kc", p=128)
                k.dma('sp', t[:, a, s, :], src, reads=['MODS'], writes=[name], allow_slow_non_contiguous=True)
        return t

    def rstd_from_ss(self, ss_ap, out_ap, n, rk, wk):
        k = self.k
        k.op('act', lambda e: e.activation(out=out_ap, in_=ss_ap, func=AF.Sqrt, scale=1.0 / n, bias=EPS), reads=rk, writes=wk)
        k.op('dve', lambda e: e.reciprocal(out=out_ap, in_=out_ap), reads=wk, writes=wk)

    def phase_norm1(self, li, hT):
        nc, k = self.nc, self.k
        with ExitStack() as st:
            mc = self.load_mod_cols(st, li, [0, 1], "mc1")
            gs = sb(st, nc, "gs1", [128, 2, 16], F32)
            for s in range(2):
                k.op('dve', lambda e, s=s: e.scalar_tensor_tensor(out=gs[:, s, :], in0=mc[:, 1, s, :], scalar=1.0, in1=self.colsb[:, C_N1G:C_N1G + 16],
                                                                  op0=ALU.add, op1=ALU.mult), reads=['mc1', 'colsb'], writes=['gs1'])
            xt = [sb(st, nc, f"n1x{i}", [128, D], F32) for i in range(4)]
            xn = [sb(st, nc, f"n1n{i}", [128, D], BF16) for i in range(4)]
            junk = sb(st, nc, "n1junk", [128, D], BF16)
            ss = sb(st, nc, "n1ss", [128, NTILE], F32)
            rs = sb(st, nc, "n1rs", [128, NTILE], F32)
            groups = [(0, 4, 0), (4, 4, 0), (8, 4, 0), (12, 4, 0), (16, 2, 1)]
            for (t0, ntl, s) in groups:
                for j in range(ntl):
                    ti = t0 + j
                    k.dma('sp', xt[j][:], self.XS[ti * 128:(ti + 1) * 128, :], reads=['XS'], writes=[f"n1x{j}"])
                    k.op('act', lambda e, j=j, ti=ti: e.activation(out=junk[:], in_=xt[j][:], func=AF.Square, accum_out=ss[:, ti:ti + 1]),
                         reads=[f"n1x{j}"], writes=['n1junk', ('n1ss', ti)])
                    self.rstd_from_ss(ss[:, ti:ti + 1], rs[:, ti:ti + 1], D, [('n1ss', ti)], [('n1rs', ti)])
                    k.op('dve', lambda e, j=j, ti=ti: e.tensor_scalar(out=xn[j][:], in0=xt[j][:], scalar1=rs[:, ti:ti + 1], scalar2=None, op0=ALU.mult),
                         reads=[f"n1x{j}", ('n1rs', ti)], writes=[f"n1n{j}"])
                tw = ntl * 128
                for kb in range(4):
                    for q in range(4):
                        kc = kb * 4 + q
                        bank = self.ps[(kc % 8)]
                        pk = f"ps{kc % 8}"
                        pv = bank[:].bitcast(BF16)
                        for j in range(ntl):
                            k.op('pe', lambda e, j=j, kc=kc, pv=pv: e.transpose(out=pv[:, j * 128:(j + 1) * 128], in_=xn[j][:, kc * 128:(kc + 1) * 128], identity=self.idb[:]),
                                 reads=[f"n1n{j}", 'idb'], writes=[pk])
                        k.op('act', lambda e, kc=kc, pv=pv, s=s, t0=t0, tw=tw: e.activation(out=hT[:, kc, t0 * 128:t0 * 128 + tw], in_=pv[:, 0:tw], func=AF.Identity,
                                                                                            scale=gs[:, s, kc:kc + 1], bias=mc[:, 0, s, kc:kc + 1]),
                             reads=[pk, 'gs1', 'mc1'], writes=[('hT', kc, t0)])

    def phase_win(self, li, hT):
        nc, k = self.nc, self.k
        segs = [
            (0, 512, PF_POOL, None), (512, 512, PF_GQ, None), (1024, 512, PF_GK, TM_GK), (1536, 512, None, TM_GV),
            (2048, 512, PF_GG, None), (2560, 32, PF_LOW, None), (2592, 512, PF_CA, None), (3104, 512, PF_CG, None),
            (3616, 512, PF_DQ, None), (4128, 512, PF_DK, None), (4640, 512, None, TM_DV)]
        wv = self.w_in[li].rearrange("(kc p) n -> p kc n", p=128)
        tgs = [(0, 512), (512, 512), (1024, 512), (1536, 512), (2048, 256)]
        hkeys = [('hT', kc, t0) for kc in range(16) for t0 in (0, 4, 8, 12, 16)]
        with ExitStack() as st:
            wb = [sb(st, nc, f"wi{i}", [128, 16, 512], BF16) for i in range(3)]
            ob = [sb(st, nc, f"wio{i}", [128, NT], F32) for i in range(2)]
            otb = [sb(st, nc, f"wit{i}", [128, 512], BF16) for i in range(3)]
            oi = 0
            oti = 0
            pi = 0
            for si, (c0, w, fm, tm) in enumerate(segs):
                W = wb[si % 3]
                wk = f"wi{si % 3}"
                k.dma('pool', W[:, :, 0:w], wv[:, :, c0:c0 + w], writes=[wk])
                if fm is not None:
                    for cc in range((w + 127) // 128):
                        m = min(128, w - cc * 128)
                        O = ob[oi % 2]
                        ok = f"wio{oi % 2}"
                        oi += 1
                        for gi, (t0, tw) in enumerate(tgs):
                            P = self.ps[pi % 4]
                            pk = f"ps{pi % 4}"
                            pi += 1
                            for kc in range(16):
                                k.op('pe', lambda e, kc=kc, P=P: e.matmul(P[0:m, 0:tw], lhsT=W[:, kc, cc * 128:cc * 128 + m], rhs=hT[:, kc, t0:t0 + tw],
                                                                       start=(kc == 0), stop=(kc == 15)), reads=[wk] + hkeys if kc == 0 else [wk], writes=[pk])
                            eng = 'act' if gi % 2 == 0 else 'dve'
                            if eng == 'act':
                                k.op('act', lambda e, P=P: e.activation(out=O[0:m, t0:t0 + tw], in_=P[0:m, 0:tw], func=AF.Copy), reads=[pk], writes=[ok])
                            else:
                                k.op('dve', lambda e, P=P: e.tensor_copy(out=O[0:m, t0:t0 + tw], in_=P[0:m, 0:tw]), reads=[pk], writes=[ok])
                        k.dma('sp', self.PF[fm + cc * 128:fm + cc * 128 + m, :], O[0:m, :], reads=[ok], writes=['PF'])
                if tm is not None:
                    for ti in range(NTILE):
                        P = self.ps[4 + pi % 4]
                        pk = f"ps{4 + pi % 4}"
                        pi += 1
                        for kc in range(16):
                            k.op('pe', lambda e, kc=kc, P=P: e.matmul(P[:, 0:512], lhsT=hT[:, kc, ti * 128:(ti + 1) * 128], rhs=W[:, kc, 0:512],
                                                                   start=(kc == 0), stop=(kc == 15)), reads=[wk] + hkeys if kc == 0 else [wk], writes=[pk])
                        O = otb[oti % 3]
                        ok = f"wit{oti % 3}"
                        oti += 1
                        if ti % 2 == 0:
                            k.op('act', lambda e, P=P, O=O: e.activation(out=O[:], in_=P[:, 0:512], func=AF.Copy), reads=[pk], writes=[ok])
                        else:
                            k.op('dve', lambda e, P=P, O=O: e.tensor_copy(out=O[:], in_=P[:, 0:512]), reads=[pk], writes=[ok])
                        k.dma('sp', self.PTM[ti * 128:(ti + 1) * 128, tm:tm + 512], O[:], reads=[ok], writes=['PTM'])

    def phase_pool(self, li):
        nc, k = self.nc, self.k
        with ExitStack() as st:
            pw = sb(st, nc, "plw", [128, 4, 128], BF16)
            k.dma('pool', pw[:], self.pool_w[li].rearrange("g c d -> c g d"), writes=['plw'])
            ic = sb(st, nc, "plic", [128, 4, NT], F32)
            for g in range(4):
                k.dma('sp', ic[:, g, :], self.invc[g:g + 1, :].broadcast_to([128, NT]), writes=[('plic', g)])
            L = T_L + 32
            U = sb(st, nc, "plU", [128, L], F32)
            A = sb(st, nc, "plA", [128, L], F32)
            B = sb(st, nc, "plB", [128, L], F32)
            pm = sb(st, nc, "plpm", [128, T_L], BF16)
            om = sb(st, nc, "plom", [128, T_L], BF16)
            pi = 0
            for g in range(4):
                for (c0, T) in ((0, T_L), (T_L, T_C)):
                    Lx = T + 32
                    k.op('pool', lambda e: e.memset(U[:, 0:Lx], 0.0), writes=['plU'])
                    k.dma('sp', U[:, 16:16 + T], self.PF[PF_POOL + g * 128:PF_POOL + (g + 1) * 128, c0:c0 + T], reads=['PF'], writes=['plU'])
                    k.op('dve', lambda e: e.tensor_tensor(out=A[:, 1:Lx], in0=U[:, 0:Lx - 1], in1=U[:, 1:Lx], op=ALU.add), reads=['plU'], writes=['plA'])
                    cur, curk = A, 'plA'
                    if g >= 1:
                        k.op('dve', lambda e: e.tensor_tensor(out=B[:, 2:Lx - 1], in0=A[:, 1:Lx - 2], in1=A[:, 3:Lx], op=ALU.add), reads=['plA'], writes=['plB'])
                        cur, curk = B, 'plB'
                    if g >= 2:
                        k.op('dve', lambda e: e.tensor_tensor(out=A[:, 4:Lx - 3], in0=B[:, 2:Lx - 5], in1=B[:, 6:Lx - 1], op=ALU.add), reads=['plB'], writes=['plA'])
                        cur, curk = A, 'plA'
                    if g >= 3:
                        k.op('dve', lambda e: e.tensor_tensor(out=B[:, 8:Lx - 7], in0=A[:, 4:Lx - 11], in1=A[:, 12:Lx - 3], op=ALU.add), reads=['plA'], writes=['plB'])
                        cur, curk = B, 'plB'
                    oth, othk = (A, 'plA') if cur is B else (B, 'plB')
                    k.op('dve', lambda e, cur=cur, oth=oth: e.tensor_tensor(out=oth[:, 16:16 + T], in0=cur[:, 16:16 + T], in1=ic[:, g, c0:c0 + T], op=ALU.mult),
                         reads=[curk, ('plic', g)], writes=[othk])
                    k.op('dve', lambda e, oth=oth: e.tensor_tensor(out=pm[:, 0:T], in0=oth[:, 16:16 + T], in1=U[:, 16:16 + T], op=ALU.subtract),
                         reads=[othk, 'plU'], writes=['plpm'])
                    for t0 in range(0, T, 512):
                        tw = min(512, T - t0)
                        P = self.ps[pi % 4]
                        pk = f"ps{pi % 4}"
                        pi += 1
                        k.op('pe', lambda e, P=P: e.matmul(P[:, 0:tw], lhsT=pw[:, g, :], rhs=pm[:, t0:t0 + tw], start=True, stop=True), reads=['plw', 'plpm'], writes=[pk])
                        k.op('act', lambda e, P=P: e.activation(out=om[:, t0:t0 + tw], in_=P[:, 0:tw], func=AF.Copy, scale=self.colsb[:, C_PSC + g:C_PSC + g + 1]),
                             reads=[pk, 'colsb'], writes=['plom'])
                    k.dma('sp', self.MT[g * 128:(g + 1) * 128, c0:c0 + T], om[:, 0:T], reads=['plom'], writes=['MT'])

    def phase_conv(self, li):
        nc, k = self.nc, self.k
        with ExitStack() as st:
            diag = sb(st, nc, "cvdiag", [128, 4, 31, 128], BF16)
            for cc in range(4):
                for d in range(31):
                    eng = 'dve'
                    k.op(eng, lambda e, cc=cc, d=d: e.tensor_scalar(out=diag[:, cc, d, :], in0=self.cfs[:, K_ID:K_ID + 128],
                                                                     scalar1=self.colsb[:, C_DW + cc * 31 + d:C_DW + cc * 31 + d + 1], scalar2=None, op0=ALU.mult),
                         reads=['cfs', 'colsb'], writes=[('cvdiag', cc, d)])
            dkeys = [('cvdiag', cc, d) for cc in range(4) for d in range(31)]
            pwb = sb(st, nc, "cvpw", [128, 4, 512], BF16)
            k.dma('pool', pwb[:], self.conv_pw[li].rearrange("(kc p) n -> p kc n", p=128), writes=['cvpw'])
            hp = sb(st, nc, "cvhp", [128, 4, T_L + 30], BF16)
            a_t = sb(st, nc, "cva", [128, T_L], F32)
            g_t = sb(st, nc, "cvg", [128, T_L], F32)
            hc = sb(st, nc, "cvhc", [128, 4, 512], F32)
            sq = sb(st, nc, "cvsq", [128, 4, 512], F32)
            mean = sb(st, nc, "cvmean", [128, 512], F32)
            m2 = sb(st, nc, "cvm2", [128, 512], F32)
            rstd = sb(st, nc, "cvrstd", [128, 512], F32)
            yb = sb(st, nc, "cvyb", [128, 4, 512], BF16)
            ytmp = sb(st, nc, "cvyt", [128, 512], F32)
            om = sb(st, nc, "cvom", [128, 4, 512], BF16)
            for (c0, T) in ((0, T_L), (T_L, T_C)):
                k.op('pool', lambda e: e.memset(hp[:], 0.0), writes=['cvhp'])
                for cc in range(4):
                    k.dma('sp', a_t[:, 0:T], self.PF[PF_CA + cc * 128:PF_CA + (cc + 1) * 128, c0:c0 + T], reads=['PF'], writes=['cva'])
                    k.dma('sp', g_t[:, 0:T], self.PF[PF_CG + cc * 128:PF_CG + (cc + 1) * 128, c0:c0 + T], reads=['PF'], writes=['cvg'])
                    k.op('act', lambda e: e.activation(out=g_t[:, 0:T], in_=g_t[:, 0:T], func=AF.Sigmoid), reads=['cvg'], writes=['cvg'])
                    k.op('dve', lambda e, cc=cc: e.tensor_tensor(out=hp[:, cc, 15:15 + T], in0=a_t[:, 0:T], in1=g_t[:, 0:T], op=ALU.mult),
                         reads=['cva', 'cvg'], writes=['cvhp'])
                for t0 in range(0, T, 512):
                    tw = min(512, T - t0)
                    for cc in range(4):
                        P = self.ps[cc % 2]
                        pk = f"ps{cc % 2}"
                        for d in range(31):
                            k.op('pe', lambda e, cc=cc, d=d, P=P: e.matmul(P[:, 0:tw], lhsT=diag[:, cc, d, :], rhs=hp[:, cc, t0 + d:t0 + d + tw], start=(d == 0), stop=(d == 30)),
                                 reads=(dkeys + ['cvhp']) if d == 0 else [], writes=[pk])
                        k.op('act', lambda e, cc=cc, P=P: e.activation(out=hc[:, cc, 0:tw], in_=P[:, 0:tw], func=AF.Identity, bias=self.colsb[:, C_CDB + cc:C_CDB + cc + 1]),
                             reads=[pk, 'colsb'], writes=[('cvhc', cc)])
                        k.op('act', lambda e, cc=cc, P=P: e.activation(out=sq[:, cc, 0:tw], in_=P[:, 0:tw], func=AF.Square, bias=self.colsb[:, C_CDB + cc:C_CDB + cc + 1]),
                             reads=[pk, 'colsb'], writes=[('cvsq', cc)])
                    P1, P2 = self.ps[2], self.ps[3]
                    for cc in range(4):
                        k.op('pe', lambda e, cc=cc: e.matmul(P1[:, 0:tw], lhsT=self.cfs[:, K_ONE:K_ONE + 128], rhs=hc[:, cc, 0:tw], start=(cc == 0), stop=(cc == 3)),
                             reads=['cfs', ('cvhc', cc)], writes=['ps2'])
                    for cc in range(4):
                        k.op('pe', lambda e, cc=cc: e.matmul(P2[:, 0:tw], lhsT=self.cfs[:, K_ONE:K_ONE + 128], rhs=sq[:, cc, 0:tw], start=(cc == 0), stop=(cc == 3)),
                             reads=['cfs', ('cvsq', cc)], writes=['ps3'])
                    k.op('act', lambda e: e.mul(out=mean[:, 0:tw], in_=P1[:, 0:tw], mul=1.0 / 512), reads=['ps2'], writes=['cvmean'])
                    k.op('dve', lambda e: e.tensor_tensor(out=m2[:, 0:tw], in0=mean[:, 0:tw], in1=mean[:, 0:tw], op=ALU.mult), reads=['cvmean'], writes=['cvm2'])
                    k.op('dve', lambda e: e.scalar_tensor_tensor(out=rstd[:, 0:tw], in0=P2[:, 0:tw], scalar=1.0 / 512, in1=m2[:, 0:tw], op0=ALU.mult, op1=ALU.subtract),
                         reads=['ps3', 'cvm2'], writes=['cvrstd'])
                    k.op('act', lambda e: e.activation(out=rstd[:, 0:tw], in_=rstd[:, 0:tw], func=AF.Sqrt, bias=EPS), reads=['cvrstd'], writes=['cvrstd'])
                    k.op('dve', lambda e: e.reciprocal(out=rstd[:, 0:tw], in_=rstd[:, 0:tw]), reads=['cvrstd'], writes=['cvrstd'])
                    for cc in range(4):
                        k.op('dve', lambda e, cc=cc: e.tensor_tensor(out=ytmp[:, 0:tw], in0=hc[:, cc, 0:tw], in1=mean[:, 0:tw], op=ALU.subtract),
                             reads=[('cvhc', cc), 'cvmean'], writes=['cvyt'])
                        k.op('dve', lambda e, cc=cc: e.tensor_tensor(out=ytmp[:, 0:tw], in0=ytmp[:, 0:tw], in1=rstd[:, 0:tw], op=ALU.mult),
                             reads=['cvyt', 'cvrstd'], writes=['cvyt'])
                        k.op('act', lambda e, cc=cc: e.activation(out=yb[:, cc, 0:tw], in_=ytmp[:, 0:tw], func=AF.Silu,
                                                                  scale=self.colsb[:, C_CLG + cc:C_CLG + cc + 1], bias=self.colsb[:, C_CLB + cc:C_CLB + cc + 1]),
                             reads=['cvyt', 'colsb'], writes=[('cvyb', cc)])
                    for dc in range(4):
                        P = self.ps[4 + dc % 2]
                        pk = f"ps{4 + dc % 2}"
                        for kc in range(4):
                            k.op('pe', lambda e, kc=kc, dc=dc, P=P: e.matmul(P[:, 0:tw], lhsT=pwb[:, kc, dc * 128:(dc + 1) * 128], rhs=yb[:, kc, 0:tw], start=(kc == 0), stop=(kc == 3)),
                                 reads=['cvpw', ('cvyb', kc)], writes=[pk])
                        k.op('act', lambda e, dc=dc, P=P: e.activation(out=om[:, dc, 0:tw], in_=P[:, 0:tw], func=AF.Identity, bias=self.colsb[:, C_CPB + dc:C_CPB + dc + 1]),
                             reads=[pk, 'colsb'], writes=[('cvom', dc)])
                        k.dma('sp', self.MT[1024 + dc * 128:1024 + (dc + 1) * 128, c0 + t0:c0 + t0 + tw], om[:, dc, 0:tw], reads=[('cvom', dc)], writes=['MT'])

    def phase_gla(self, li):
        nc, k = self.nc, self.k
        HP = 2
        with ExitStack() as st:
            LA = [sb(st, nc, f"glLA{d}", [17, NT], F32) for d in range(2)]
            UP = [sb(st, nc, f"glUP{d}", [17, 512], F32) for d in range(2)]
            for d in range(2):
                k.op('pool', lambda e, d=d: e.memset(LA[d][:], 1.0), writes=[f"glLA{d}"])
                k.dma('sp', LA[d][0:16, :], self.PF[PF_LOW + 16 * d:PF_LOW + 16 * (d + 1), :], reads=['PF'], writes=[f"glLA{d}"])
                k.dma('sp', UP[d][:], (self.up_f if d == 0 else self.up_b)[li], writes=[f"glUP{d}"])
            sg = sb(st, nc, "glsg", [128, NT], F32)
            k.dma('sp', sg[:], self.segm, writes=['glsg'])
            for hp in range(0, 4, HP):
                heads = list(range(hp, hp + HP))
                W = HP * 128
                with ExitStack() as st2:
                    KH = [sb(st2, nc, f"glKH{d}", [128, NTILE, W], BF16) for d in range(2)]
                    V = sb(st2, nc, "glV", [128, NTILE, W], BF16)
                    Kt = sb(st2, nc, "glKt", [128, NTILE, W], BF16)
                    for ti in range(NTILE):
                        k.dma('sp', V[:, ti, :], self.PTM[ti * 128:(ti + 1) * 128, TM_GV + hp * 128:TM_GV + hp * 128 + W], reads=['PTM'], writes=['glV'])
                        k.dma('sp', Kt[:, ti, :], self.PTM[ti * 128:(ti + 1) * 128, TM_GK + hp * 128:TM_GK + hp * 128 + W], reads=['PTM'], writes=['glKt'])
                    spts = [sb(st2, nc, f"glspt{i}", [128, W], F32) for i in range(3)]
                    decs = [sb(st2, nc, f"gldec{i}", [128, W], F32) for i in range(3)]
                    rr = 0
                    for ti in range(NTILE):
                        for d in range(2):
                            P = self.ps[d]
                            spt = spts[rr % 3]
                            dec = decs[rr % 3]
                            sk = f"glspt{rr % 3}"
                            dk2 = f"gldec{rr % 3}"
                            rr += 1
                            k.op('pe', lambda e, d=d, P=P: e.matmul(P[:, 0:W], lhsT=LA[d][:, ti * 128:(ti + 1) * 128], rhs=UP[d][:, hp * 128:hp * 128 + W], start=True, stop=True),
                                 reads=[f"glLA{d}", f"glUP{d}"], writes=[f"ps{d}"])
                            k.op('act', lambda e, P=P, spt=spt: e.activation(out=spt[:], in_=P[:, 0:W], func=AF.Exp, scale=-1.0), reads=[f"ps{d}"], writes=[sk])
                            k.op('act', lambda e, spt=spt: e.activation(out=spt[:], in_=spt[:], func=AF.Ln, bias=1.0), reads=[sk], writes=[sk])
                            P2 = self.ps[2 + d]
                            tri = K_UF if d == 0 else K_UB
                            k.op('pe', lambda e, P2=P2, tri=tri, spt=spt: e.matmul(P2[:, 0:W], lhsT=self.cfs[:, tri:tri + 128], rhs=spt[:], start=True, stop=True),
                                 reads=['cfs', sk], writes=[f"ps{2 + d}"])
                            k.op('act', lambda e, P2=P2, dec=dec: e.activation(out=dec[:], in_=P2[:, 0:W], func=AF.Exp, scale=-1.0 / 16), reads=[f"ps{2 + d}"], writes=[dk2])
                            k.op('dve', lambda e, d=d, dec=dec: e.tensor_tensor(out=KH[d][:, ti, :], in0=Kt[:, ti, :], in1=dec[:], op=ALU.mult),
                                 reads=['glKt', dk2], writes=[(f"glKH{d}", ti)])
                    qt = {}
                    kt = {}
                    dcy = {}
                    S = {}
                    Sb = {}
                    O = {}
                    raw_q = sb(st2, nc, "glrq", [128, NT], F32)
                    raw_k = sb(st2, nc, "glrk", [128, NT], F32)
                    spT = sb(st2, nc, "glspT", [128, NT], F32)
                    Gs = sb(st2, nc, "glGs", [128, NT], F32)
                    E1 = sb(st2, nc, "glE1", [128, NT], F32)
                    tgs = [(0, 512), (512, 512), (1024, 512), (1536, 512), (2048, 256)]
                    for h in heads:
                        O[h] = sb(st2, nc, f"glO{h}", [128, NT], F32)
                        k.dma('sp', raw_q[:], self.PF[PF_GQ + h * 128:PF_GQ + (h + 1) * 128, :], reads=['PF'], writes=['glrq'])
                        k.dma('sp', raw_k[:], self.PF[PF_GK + h * 128:PF_GK + (h + 1) * 128, :], reads=['PF'], writes=['glrk'])
                        for d in range(2):
                            c = (h, d)
                            qt[c] = sb(st2, nc, f"glqt{h}{d}", [128, NT], BF16)
                            kt[c] = sb(st2, nc, f"glkt{h}{d}", [128, NT], BF16)
                            dcy[c] = sb(st2, nc, f"gldc{h}{d}", [128, NT // 64], F32)
                            S[c] = sb(st2, nc, f"glS{h}{d}", [128, 128], F32)
                            Sb[c] = sb(st2, nc, f"glSb{h}{d}", [128, 128], BF16)
                            k.op('pool', lambda e, c=c: e.memset(S[c][:], 0.0), writes=[('glS', c)])
                            k.op('pool', lambda e, c=c: e.memset(Sb[c][:], 0.0), writes=[('glSb', c)])
                            for gi, (t0, tw) in enumerate(tgs):
                                P = self.ps[4 + gi % 2]
                                pk = f"ps{4 + gi % 2}"
                                k.op('pe', lambda e, P=P, d=d, h=h: e.matmul(P[:, 0:tw], lhsT=UP[d][:, h * 128:(h + 1) * 128], rhs=LA[d][:, t0:t0 + tw], start=True, stop=True),
                                     reads=[f"glLA{d}", f"glUP{d}"], writes=[pk])
                                k.op('act', lambda e, P=P: e.activation(out=spT[:, t0:t0 + tw], in_=P[:, 0:tw], func=AF.Exp, scale=-1.0), reads=[pk], writes=['glspT'])
                            k.op('act', lambda e: e.activation(out=spT[:], in_=spT[:], func=AF.Ln, bias=1.0), reads=['glspT'], writes=['glspT'])
                            k.op('dve', lambda e: e.tensor_tensor_scan(out=Gs[:], data0=sg[:], data1=spT[:], initial=0.0, op0=ALU.mult, op1=ALU.add),
                                 reads=['glsg', 'glspT'], writes=['glGs'])
                            k.op('act', lambda e, c=c: e.activation(out=dcy[c][:], in_=Gs[:, 63:NT:64], func=AF.Exp, scale=-1.0 / 16), reads=['glGs'], writes=[('gldc', c)])
                            if d == 1:
                                k.op('dve', lambda e: e.tensor_tensor(out=E1[:], in0=spT[:], in1=Gs[:], op=ALU.subtract), reads=['glspT', 'glGs'], writes=['glE1'])
                                E1v = E1[:].rearrange("p (c j) -> p c j", j=64)
                                totv = Gs[:].rearrange("p (c j) -> p c j", j=64)[:, :, 63:64].broadcast_to([128, NT // 64, 64])
                                k.op('dve', lambda e: e.tensor_tensor(out=E1v, in0=E1v, in1=totv, op=ALU.add), reads=['glE1', 'glGs'], writes=['glE1'])
                                k.op('dve', lambda e: e.tensor_copy(out=Gs[:], in_=E1[:]), reads=['glE1'], writes=['glGs'])
                            k.op('act', lambda e: e.activation(out=E1[:], in_=Gs[:], func=AF.Exp, scale=-1.0 / 16), reads=['glGs'], writes=['glE1'])
                            k.op('dve', lambda e, c=c: e.scalar_tensor_tensor(out=qt[c][:], in0=raw_q[:], scalar=128.0 ** -0.5, in1=E1[:], op0=ALU.mult, op1=ALU.mult),
                                 reads=['glrq', 'glE1'], writes=[('glqt', c)])
                            k.op('act', lambda e: e.activation(out=E1[:], in_=Gs[:], func=AF.Exp, scale=1.0 / 16), reads=['glGs', ('glqt', c)], writes=['glE1'])
                            k.op('dve', lambda e, c=c: e.tensor_tensor(out=kt[c][:], in0=raw_k[:], in1=E1[:], op=ALU.mult), reads=['glrk', 'glE1'], writes=[('glkt', c)])
                    attm = {c: sb(st2, nc, f"glam{c[0]}{c[1]}", [128, 128], BF16) for c in qt}
                    combos = [(h, d) for h in heads for d in range(2)]
                    pso = {c: self.ps[i] for i, c in enumerate(combos)}
                    psok = {c: f"ps{i}" for i, c in enumerate(combos)}
                    order_f = [16, 17] + list(range(16))
                    order_b = [17, 16] + list(range(15, -1, -1))
                    for step in range(NTILE):
                        for half in range(2):
                            for c in combos:
                                h, d = c
                                hh = h - hp
                                ti = order_f[step] if d == 0 else order_b[step]
                                tcols = slice(ti * 128, (ti + 1) * 128)
                                ch = half if d == 0 else 1 - half
                                rows = slice(ch * 64, ch * 64 + 64)
                                ccols = slice(ti * 128 + ch * 64, ti * 128 + ch * 64 + 64)
                                cidx = ti * 2 + ch
                                if half == 0:
                                    PA = self.ps[4 + (combos.index(c) % 2)]
                                    pak = f"ps{4 + (combos.index(c) % 2)}"
                                    k.op('pe', lambda e, c=c, PA=PA, tcols=tcols: e.matmul(PA[:, 0:128], lhsT=kt[c][:, tcols], rhs=qt[c][:, tcols], start=True, stop=True),
                                         reads=[('glkt', c), ('glqt', c)], writes=[pak])
                                    mk = K_MF if d == 0 else K_MB
                                    k.op('dve', lambda e, c=c, PA=PA, mk=mk: e.tensor_tensor(out=attm[c][:], in0=PA[:, 0:128], in1=self.cfs[:, mk:mk + 128], op=ALU.mult),
                                         reads=[pak, 'cfs'], writes=[('glam', c)])
                                    k.op('pe', lambda e, c=c, ti=ti, hh=hh: e.matmul(pso[c][:, 0:128], lhsT=V[:, ti, hh * 128:(hh + 1) * 128], rhs=attm[c][:], start=True, stop=False),
                                         reads=['glV', ('glam', c)], writes=[psok[c]])
                                k.op('pe', lambda e, c=c, ch=ch, ccols=ccols, half=half: e.matmul(pso[c][:, ch * 64:ch * 64 + 64], lhsT=Sb[c][:], rhs=qt[c][:, ccols], start=False, stop=(half == 1)),
                                     reads=[('glSb', c), ('glqt', c)], writes=[psok[c]])
                                PS_ = self.ps[6 + (combos.index(c) % 2)]
                                psk = f"ps{6 + (combos.index(c) % 2)}"
                                k.op('pe', lambda e, c=c, PS_=PS_, rows=rows, ti=ti, hh=hh, d=d: e.matmul(PS_[:, 0:128], lhsT=KH[d][rows, ti, hh * 128:(hh + 1) * 128],
                                                                                                         rhs=V[rows, ti, hh * 128:(hh + 1) * 128], start=True, stop=True),
                                     reads=[(f"glKH{d}", ti), 'glV'], writes=[psk])
                                k.op('dve', lambda e, c=c, PS_=PS_, cidx=cidx: e.scalar_tensor_tensor(out=S[c][:], in0=S[c][:], scalar=dcy[c][:, cidx:cidx + 1], in1=PS_[:, 0:128],
                                                                                                      op0=ALU.mult, op1=ALU.add),
                                     reads=[psk, ('glS', c), ('gldc', c)], writes=[('glS', c)])
                                k.op('act', lambda e, c=c: e.activation(out=Sb[c][:], in_=S[c][:], func=AF.Copy), reads=[('glS', c)], writes=[('glSb', c)])
                                if half == 1:
                                    other = order_b if d == 0 else order_f
                                    if step < other.index(ti):
                                        k.op('act', lambda e, c=c, tcols=tcols, h=h: e.activation(out=O[h][:, tcols], in_=pso[c][:, 0:128], func=AF.Copy),
                                             reads=[psok[c]], writes=[('glO', h, ti)])
                                    else:
                                        k.op('dve', lambda e, c=c, tcols=tcols, h=h: e.tensor_tensor(out=O[h][:, tcols], in0=pso[c][:, 0:128], in1=O[h][:, tcols], op=ALU.add),
                                             reads=[psok[c], ('glO', h, ti)], writes=[('glO', h, ti)])
                    gt = raw_q
                    sqb = raw_k
                    for h in heads:
                        okeys = [('glO', h, ti) for ti in range(NTILE)]
                        k.dma('sp', gt[:], self.PF[PF_GG + h * 128:PF_GG + (h + 1) * 128, :], reads=['PF'], writes=['glrq'])
                        k.op('act', lambda e: e.activation(out=gt[:], in_=gt[:], func=AF.Silu), reads=['glrq'], writes=['glrq'])
                        k.op('act', lambda e, h=h: e.activation(out=sqb[:], in_=O[h][:], func=AF.Square), reads=okeys, writes=['glrk'])
                        for gi, (t0, tw) in enumerate(tgs):
                            P = self.ps[4 + gi % 2]
                            pk = f"ps{4 + gi % 2}"
                            k.op('pe', lambda e, P=P: e.matmul(P[:, 0:tw], lhsT=self.cfs[:, K_ONE:K_ONE + 128], rhs=sqb[:, t0:t0 + tw], start=True, stop=True),
                                 reads=['cfs', 'glrk'], writes=[pk])
                            k.op('dve', lambda e, P=P: e.tensor_scalar(out=Gs[:, t0:t0 + tw], in0=P[:, 0:tw], scalar1=1.0 / 128, scalar2=EPS, op0=ALU.mult, op1=ALU.add),
                                 reads=[pk], writes=['glGs'])
                        k.op('act', lambda e: e.activation(out=Gs[:], in_=Gs[:], func=AF.Sqrt), reads=['glGs'], writes=['glGs'])
                        k.op('dve', lambda e: e.reciprocal(out=Gs[:], in_=Gs[:]), reads=['glGs'], writes=['glGs'])
                        k.op('dve', lambda e, h=h: e.scalar_tensor_tensor(out=E1[:], in0=O[h][:], scalar=self.colsb[:, C_GNG:C_GNG + 1], in1=Gs[:], op0=ALU.mult, op1=ALU.mult),
                             reads=okeys + ['glGs', 'colsb'], writes=['glE1'])
                        ob = qt[(h, 0)]
                        k.op('dve', lambda e, ob=ob: e.tensor_tensor(out=ob[:], in0=E1[:], in1=gt[:], op=ALU.mult), reads=['glE1', 'glrq'], writes=[('glqt', (h, 0))])
                        k.dma('sp', self.MT[512 + h * 128:512 + (h + 1) * 128, :], ob[:], reads=[('glqt', (h, 0))], writes=['MT'])
                k.barrier()

    def phase_attn(self, li):
        nc, k = self.nc, self.k
        with ExitStack() as st:
            rC = sb(st, nc, "atC", [128, T_L], F32)
            rS = sb(st, nc, "atS", [128, T_L], F32)
            k.dma('sp', rC[:], self.ropeC, writes=['atC'])
            k.dma('sp', rS[:], self.ropeS, writes=['atS'])
            lam = sb(st, nc, "atlam", [128, 4], F32)
            tmp64 = sb(st, nc, "attmp64", [128, 64], F32)
            for j in range(2):
                k.op('dve', lambda e, j=j: e.tensor_tensor(out=tmp64[:], in0=self.colsb[:, C_LQ + 128 * j:C_LQ + 128 * j + 64], in1=self.colsb[:, C_LQ + 128 * j + 64:C_LQ + 128 * j + 128], op=ALU.mult),
                     reads=['colsb'], writes=['attmp64'])
                k.op('dve', lambda e, j=j: e.reduce_sum(out=lam[:, j:j + 1], in_=tmp64[:], axis=AX.X), reads=['attmp64'], writes=['atlam'])
            k.op('act', lambda e: e.activation(out=lam[:, 0:2], in_=lam[:, 0:2], func=AF.Exp), reads=['atlam'], writes=['atlam'])
            k.op('dve', lambda e: e.tensor_tensor(out=lam[:, 2:3], in0=lam[:, 0:1], in1=lam[:, 1:2], op=ALU.subtract), reads=['atlam'], writes=['atlam'])
            k.op('dve', lambda e: e.tensor_tensor(out=lam[:, 2:3], in0=lam[:, 2:3], in1=self.colsb[:, C_LAMI:C_LAMI + 1], op=ALU.add), reads=['atlam', 'colsb'], writes=['atlam'])
            k.op('dve', lambda e: e.tensor_scalar(out=lam[:, 3:4], in0=lam[:, 2:3], scalar1=-1.0, scalar2=None, op0=ALU.mult), reads=['atlam'], writes=['atlam'])
            sgc = sb(st, nc, "atsgc", [128, 1], F32)
            k.op('dve', lambda e: e.tensor_tensor(out=sgc[:], in0=self.colsb[:, C_SLG:C_SLG + 1], in1=self.colsb[:, C_OML:C_OML + 1], op=ALU.mult), reads=['colsb'], writes=['atsgc'])
            raws = [[sb(st, nc, f"atraw{p}{w}", [128, NT], F32) for w in range(2)] for p in range(2)]
            t1s = [sb(st, nc, f"att1{p}", [128, 512], F32) for p in range(2)]
            t2s = [sb(st, nc, f"att2{p}", [128, 512], F32) for p in range(2)]
            qrs = [sb(st, nc, f"atqr{p}", [128, NT], BF16) for p in range(2)]
            krs = [sb(st, nc, f"atkr{p}", [128, NT], BF16) for p in range(2)]
            Vas = [sb(st, nc, f"atVa{p}", [128, NTILE, 129], BF16) for p in range(2)]
            ptb = [sb(st, nc, f"atpt{i}", [128, 512], BF16) for i in range(3)]
            dsb = sb(st, nc, "atd", [128, 4, 128], F32)
            accsb = [[sb(st, nc, f"atacc{m}{q}", [128, 129], F32) for q in range(4)] for m in range(2)]
            rd = sb(st, nc, "atrd", [128, 8], F32)
            ssq = sb(st, nc, "atssq", [128, 4], F32)
            junk = sb(st, nc, "atjunk", [128, 128], F32)
            dn = sb(st, nc, "atdn", [128, 4, 128], BF16)
            omd = sb(st, nc, "atomd", [128, NT], BF16)
            pti = 0

            def prep_pieces(h):
                p = h % 2
                pieces = []

                def loads():
                    for w, prow in ((0, PF_DQ), (1, PF_DK)):
                        k.dma('sp', raws[p][w][:], self.PF[prow + h * 128:prow + (h + 1) * 128, :], reads=['PF'], writes=[f"atraw{p}{w}"])
                    k.op('pool', lambda e: e.memset(Vas[p][:], 1.0), writes=[f"atVa{p}"])
                    for ti in range(NTILE):
                        k.dma('sp', Vas[p][:, ti, 0:128], self.PTM[ti * 128:(ti + 1) * 128, TM_DV + h * 128:TM_DV + (h + 1) * 128], reads=['PTM'], writes=[f"atVa{p}"])
                pieces.append(loads)
                for w in range(2):
                    dst = qrs[p] if w == 0 else krs[p]
                    dk_ = (f"atqr{p}" if w == 0 else f"atkr{p}")
                    raw = raws[p][w]
                    rk = f"atraw{p}{w}"
                    for gi in range(4):
                        def piece(gi=gi, dst=dst, dk_=dk_, raw=raw, rk=rk):
                            t0 = gi * 512
                            P = self.ps[6 + gi % 2]
                            pk = f"ps{6 + gi % 2}"
                            t1 = t1s[gi % 2]
                            t2 = t2s[gi % 2]
                            k.op('pe', lambda e: e.matmul(P[:, :], lhsT=self.cfs[:, K_PM:K_PM + 128], rhs=raw[:, t0:t0 + 512], start=True, stop=True),
                                 reads=['cfs', rk], writes=[pk])
                            k.op('dve', lambda e: e.tensor_tensor(out=t1[:], in0=P[:, :], in1=rS[:, t0:t0 + 512], op=ALU.mult), reads=[pk, 'atS'], writes=[f"att1{gi % 2}"])
                            k.op('pool', lambda e: e.tensor_tensor(out=t2[:], in0=raw[:, t0:t0 + 512], in1=rC[:, t0:t0 + 512], op=ALU.mult), reads=[rk, 'atC'], writes=[f"att2{gi % 2}"])
                            k.op('dve', lambda e: e.tensor_tensor(out=dst[:, t0:t0 + 512], in0=t1[:], in1=t2[:], op=ALU.add), reads=[f"att1{gi % 2}", f"att2{gi % 2}"], writes=[dk_])
                        pieces.append(piece)

                    def ctxcopy(dst=dst, dk_=dk_, raw=raw, rk=rk):
                        k.op('pool', lambda e: e.tensor_copy(out=dst[:, T_L:NT], in_=raw[:, T_L:NT]), reads=[rk], writes=[dk_])
                    pieces.append(ctxcopy)
                return pieces

            for pc in prep_pieces(0):
                pc()
            for h in range(4):
                qr, kr, Va = qrs[h % 2], krs[h % 2], Vas[h % 2]
                qrk, krk, vak = f"atqr{h % 2}", f"atkr{h % 2}", f"atVa{h % 2}"
                nxt = prep_pieces(h + 1) if h + 1 < 4 else []
                if nxt:
                    nxt.pop(0)()
                qgroups = [(0, 512, list(range(NTILE))), (512, 512, list(range(NTILE))), (1024, 512, list(range(NTILE))), (1536, 512, list(range(NTILE))), (2048, 256, [16, 17])]
                for (q0, qw, ktiles) in qgroups:
                    nq = qw // 128
                    for m in range(2):
                        mr = slice(m * 64, m * 64 + 64)
                        accs = [self.ps[2], self.ps[3], self.ps[4], self.ps[5]]
                        acck = ["ps2", "ps3", "ps4", "ps5"]
                        nk = len(ktiles)

                        def emit_S(ki):
                            P = self.ps[ki % 2]
                            kt_ = ktiles[ki]
                            k.op('pe', lambda e: e.matmul(P[:, 0:qw], lhsT=kr[mr, kt_ * 128:(kt_ + 1) * 128], rhs=qr[mr, q0:q0 + qw], start=True, stop=True),
                                 reads=[krk, qrk], writes=[f"ps{ki % 2}"])

                        emit_S(0)
                        for ki, kt_ in enumerate(ktiles):
                            if ki + 1 < nk:
                                emit_S(ki + 1)
                            P = self.ps[ki % 2]
                            pk = f"ps{ki % 2}"
                            pt = ptb[pti % 3]
                            ptk = f"atpt{pti % 3}"
                            pti += 1
                            k.op('act', lambda e, P=P, pt=pt: e.activation(out=pt[:, 0:qw], in_=P[:, 0:qw], func=AF.Exp, scale=0.125), reads=[pk], writes=[ptk])
                            for qt_ in range(nq):
                                acc = accs[qt_]
                                ak = acck[qt_]
                                k.op('pe', lambda e, acc=acc, pt=pt, qt_=qt_, kt_=kt_, ki=ki: e.matmul(acc[:, 0:129], lhsT=pt[:, qt_ * 128:(qt_ + 1) * 128], rhs=Va[:, kt_, :],
                                                                                                 start=(ki == 0), stop=(ki == nk - 1)),
                                     reads=[ptk, vak], writes=[ak])
                        for qt_ in range(nq):
                            acc = accs[qt_]
                            ak = acck[qt_]
                            col = m * 4 + qt_
                            asb = accsb[m][qt_]
                            ask = ('ataccsb', m, qt_)
                            k.op('act', lambda e, acc=acc, asb=asb: e.activation(out=asb[:], in_=acc[:, 0:129], func=AF.Copy), reads=[ak], writes=[ask])
                            k.op('dve', lambda e, asb=asb, col=col: e.reciprocal(out=rd[:, col:col + 1], in_=asb[:, 128:129]), reads=[ask], writes=[('atrd', col)])
                            if m == 0:
                                k.op('dve', lambda e, asb=asb, col=col, qt_=qt_: e.tensor_scalar(out=dsb[:, qt_, :], in0=asb[:, 0:128], scalar1=rd[:, col:col + 1], scalar2=None, op0=ALU.mult),
                                     reads=[ask, ('atrd', col)], writes=[('atd', qt_)])
                            else:
                                k.op('dve', lambda e, col=col: e.tensor_tensor(out=rd[:, col:col + 1], in0=rd[:, col:col + 1], in1=lam[:, 3:4], op=ALU.mult),
                                     reads=[('atrd', col), 'atlam'], writes=[('atrd', col)])
                                k.op('dve', lambda e, asb=asb, col=col, qt_=qt_: e.scalar_tensor_tensor(out=dsb[:, qt_, :], in0=asb[:, 0:128], scalar=rd[:, col:col + 1], in1=dsb[:, qt_, :],
                                                                                                    op0=ALU.mult, op1=ALU.add),
                                     reads=[ask, ('atrd', col), ('atd', qt_)], writes=[('atd', qt_)])
                        if nxt:
                            nxt.pop(0)()
                    for qt_ in range(nq):
                        k.op('act', lambda e, qt_=qt_: e.activation(out=junk[:], in_=dsb[:, qt_, :], func=AF.Square, accum_out=ssq[:, qt_:qt_ + 1]),
                             reads=[('atd', qt_)], writes=['atjunk', ('atssq', qt_)])
                        self.rstd_from_ss(ssq[:, qt_:qt_ + 1], ssq[:, qt_:qt_ + 1], 128, [('atssq', qt_)], [('atssq', qt_)])
                        k.op('dve', lambda e, qt_=qt_: e.tensor_scalar(out=dn[:, qt_, :], in0=dsb[:, qt_, :], scalar1=ssq[:, qt_:qt_ + 1], scalar2=None, op0=ALU.mult),
                             reads=[('atd', qt_), ('atssq', qt_)], writes=[('atdn', qt_)])
                        pv = self.ps[6 + qt_ % 2][:].bitcast(BF16)
                        pk = f"ps{6 + qt_ % 2}"
                        k.op('pe', lambda e, pv=pv, qt_=qt_: e.transpose(out=pv[:, 0:128], in_=dn[:, qt_, :], identity=self.idb[:]), reads=[('atdn', qt_), 'idb'], writes=[pk])
                        k.op('act', lambda e, pv=pv, qt_=qt_: e.activation(out=omd[:, q0 + qt_ * 128:q0 + (qt_ + 1) * 128], in_=pv[:, 0:128], func=AF.Copy, scale=sgc[:, 0:1]),
                             reads=[pk, 'atsgc'], writes=['atomd'])
                while nxt:
                    nxt.pop(0)()
                k.dma('sp', self.MT[1536 + h * 128:1536 + (h + 1) * 128, :], omd[:], reads=['atomd'], writes=['MT'])

    def phase_wout(self, li):
        nc, k = self.nc, self.k
        with ExitStack() as st:
            mt = sb(st, nc, "womt", [128, 16, NT], BF16)
            for kc in range(16):
                k.dma('sp', mt[:, kc, :], self.MT[kc * 128:(kc + 1) * 128, :], reads=['MT'], writes=[('womt', kc)])
            mkeys = [('womt', kc) for kc in range(16)]
            wsl = [sb(st, nc, f"wow{i}", [128, 16, 512], BF16) for i in range(2)]
            gb = sb(st, nc, "wogb", [128, 2, D], F32)
            for s in range(2):
                k.dma('sp', gb[:, s, :], self.MODS[li, s:s + 1, 2 * D:3 * D].broadcast_to([128, D]), reads=['MODS'], writes=['wogb'])
            xt = [sb(st, nc, f"wox{i}", [128, 512], F32) for i in range(3)]
            yt = [sb(st, nc, f"woy{i}", [128, 512], F32) for i in range(3)]
            wv = self.w_out[li].rearrange("(kc p) n -> p kc n", p=128)
            it = 0
            for dg in range(4):
                W = wsl[dg % 2]
                wk = f"wow{dg % 2}"
                k.dma('pool', W[:], wv[:, :, dg * 512:(dg + 1) * 512], writes=[wk])
                for ti in range(NTILE):
                    s = 0 if ti < 16 else 1
                    X = xt[it % 3]
                    xk = f"wox{it % 3}"
                    Y = yt[it % 3]
                    yk = f"woy{it % 3}"
                    P = self.ps[it % 4]
                    pk = f"ps{it % 4}"
                    it += 1
                    reg = ('XS', ti, dg)
                    k.dma('sp', X[:], self.XS[ti * 128:(ti + 1) * 128, dg * 512:(dg + 1) * 512], reads=['XS', reg], writes=[xk])
                    for kc in range(16):
                        k.op('pe', lambda e, kc=kc, P=P, W=W: e.matmul(P[:, :], lhsT=mt[:, kc, ti * 128:(ti + 1) * 128], rhs=W[:, kc, :], start=(kc == 0), stop=(kc == 15)),
                             reads=([wk] + mkeys) if kc == 0 else [], writes=[pk])
                    k.op('dve', lambda e, P=P, Y=Y, s=s: e.tensor_tensor(out=Y[:], in0=P[:, :], in1=gb[:, s, dg * 512:(dg + 1) * 512], op=ALU.mult), reads=[pk, 'wogb'], writes=[yk])
                    k.op('pool', lambda e, X=X, Y=Y: e.tensor_tensor(out=Y[:], in0=Y[:], in1=X[:], op=ALU.add), reads=[yk, xk], writes=[yk])
                    k.dma('sp', self.XS[ti * 128:(ti + 1) * 128, dg * 512:(dg + 1) * 512], Y[:], reads=[yk], writes=[reg])

    def phase_norm2(self, li):
        nc, k = self.nc, self.k
        with ExitStack() as st:
            mc = self.load_mod_cols(st, li, [3, 4], "mc2")
            gs = sb(st, nc, "gs2", [128, 2, 16], F32)
            for s in range(2):
                k.op('dve', lambda e, s=s: e.scalar_tensor_tensor(out=gs[:, s, :], in0=mc[:, 1, s, :], scalar=1.0, in1=self.colsb[:, C_N2G:C_N2G + 16],
                                                                  op0=ALU.add, op1=ALU.mult), reads=['mc2', 'colsb'], writes=['gs2'])
            wr = sb(st, nc, "n2wr", [128, 16, NE], F32)
            k.dma('sp', wr[:], self.w_router[li].rearrange("(kc p) e -> p kc e", p=128), writes=['n2wr'])
            xt = [sb(st, nc, f"n2x{i}", [128, D], F32) for i in range(2)]
            xnf = [sb(st, nc, f"n2f{i}", [128, D], F32) for i in range(2)]
            xnb = [sb(st, nc, f"n2b{i}", [128, D], BF16) for i in range(2)]
            junk = sb(st, nc, "n2junk", [128, D], BF16)
            ss = sb(st, nc, "n2ss", [128, NTILE], F32)
            rs = sb(st, nc, "n2rs", [128, NTILE], F32)
            h2T = [sb(st, nc, f"n2h{i}", [128, 16, 128], F32) for i in range(2)]
            ex = sb(st, nc, "n2ex", [NE, NT], F32)
            rcp = sb(st, nc, "n2rcp", [NE, 512], F32)
            for ti in range(NTILE):
                s = 0 if ti < 16 else 1
                j = ti % 2
                k.dma('sp', xt[j][:], self.XS[ti * 128:(ti + 1) * 128, :], reads=['XS'], writes=[f"n2x{j}"])
                k.op('act', lambda e, j=j, ti=ti: e.activation(out=junk[:], in_=xt[j][:], func=AF.Square, accum_out=ss[:, ti:ti + 1]),
                     reads=[f"n2x{j}"], writes=['n2junk', ('n2ss', ti)])
                self.rstd_from_ss(ss[:, ti:ti + 1], rs[:, ti:ti + 1], D, [('n2ss', ti)], [('n2rs', ti)])
                k.op('dve', lambda e, j=j, ti=ti: e.tensor_scalar(out=xnf[j][:], in0=xt[j][:], scalar1=rs[:, ti:ti + 1], scalar2=None, op0=ALU.mult),
                     reads=[f"n2x{j}", ('n2rs', ti)], writes=[f"n2f{j}"])
                k.op('pool', lambda e, j=j: e.tensor_copy(out=xnb[j][:], in_=xnf[j][:]), reads=[f"n2f{j}"], writes=[f"n2b{j}"])
                k.dma('sp', self.XN2[ti * 128:(ti + 1) * 128, :], xnb[j][:], reads=[f"n2b{j}"], writes=['XN2'])
                for kb in range(4):
                    P = self.ps[kb % 4]
                    pk = f"ps{kb % 4}"
                    for q in range(4):
                        kc = kb * 4 + q
                        k.op('pe', lambda e, P=P, q=q, kc=kc, j=j: e.matmul(P[:, q * 128:(q + 1) * 128], lhsT=xnf[j][:, kc * 128:(kc + 1) * 128], rhs=self.cfs[:, K_ID:K_ID + 128], start=True, stop=True),
                             reads=[f"n2f{j}", 'cfs'], writes=[pk])
                    for q in range(4):
                        kc = kb * 4 + q
                        k.op('act', lambda e, P=P, q=q, kc=kc, j=j, s=s: e.activation(out=h2T[j][:, kc, :], in_=P[:, q * 128:(q + 1) * 128], func=AF.Identity,
                                                                                    scale=gs[:, s, kc:kc + 1], bias=mc[:, 0, s, kc:kc + 1]),
                             reads=[pk, 'gs2', 'mc2'], writes=[(f"n2h{j}", kc)])
                PL = self.ps[4 + ti % 2]
                plk = f"ps{4 + ti % 2}"
                for kc in range(16):
                    k.op('pe', lambda e, kc=kc, PL=PL, j=j: e.matmul(PL[0:NE, 0:128], lhsT=wr[:, kc, :], rhs=h2T[j][:, kc, :], start=(kc == 0), stop=(kc == 15)),
                         reads=['n2wr', (f"n2h{j}", kc)], writes=[plk])
                k.op('act', lambda e, PL=PL, ti=ti: e.activation(out=ex[:, ti * 128:(ti + 1) * 128], in_=PL[0:NE, 0:128], func=AF.Exp), reads=[plk], writes=['n2ex'])
            for gi, (t0, tw) in enumerate([(0, 512), (512, 512), (1024, 512), (1536, 512), (2048, 256)]):
                P = self.ps[6 + gi % 2]
                pk = f"ps{6 + gi % 2}"
                k.op('pe', lambda e, P=P: e.matmul(P[0:NE, 0:tw], lhsT=self.cfs[0:NE, K_ONE:K_ONE + NE], rhs=ex[:, t0:t0 + tw], start=True, stop=True),
                     reads=['cfs', 'n2ex'], writes=[pk])
                k.op('dve', lambda e, P=P: e.reciprocal(out=rcp[:, 0:tw], in_=P[0:NE, 0:tw]), reads=[pk], writes=['n2rcp'])
                k.op('dve', lambda e, P=P: e.tensor_tensor(out=ex[:, t0:t0 + tw], in0=ex[:, t0:t0 + tw], in1=rcp[:, 0:tw], op=ALU.mult), reads=['n2rcp', 'n2ex'], writes=['n2ex'])
            k.dma('sp', self.AFF, ex[:], reads=['n2ex'], writes=['AFF'])

    def phase_moe(self, li):
        nc, k = self.nc, self.k
        with ExitStack() as st:
            mc = self.load_mod_cols(st, li, [3, 4], "mc3")
            gs = sb(st, nc, "gs3", [128, 2, 16], F32)
            for s in range(2):
                k.op('dve', lambda e, s=s: e.scalar_tensor_tensor(out=gs[:, s, :], in0=mc[:, 1, s, :], scalar=1.0, in1=self.colsb[:, C_N2G:C_N2G + 16],
                                                                  op0=ALU.add, op1=ALU.mult), reads=['mc3', 'colsb'], writes=['gs3'])
            vals = sb(st, nc, "mevals", [NE, CAP_L + CAP_C], F32)
            idxu = sb(st, nc, "meidxu", [NE, CAP_L + CAP_C], U32)
            idxf = sb(st, nc, "meidxf", [NE, CAP_L + CAP_C], F32)
            gcol = sb(st, nc, "megcol", [128, 3, NE], F32)
            icol = sb(st, nc, "meicol", [128, 3, NE], I32)
            with ExitStack() as st2:
                aff = sb(st2, nc, "meaff", [NE, NT], F32)
                wk_ = sb(st2, nc, "mewk", [NE, NT], F32)
                k.dma('sp', aff[:], self.AFF, reads=['AFF'], writes=['meaff'])
                for (c0, T, cap, o0) in ((0, T_L, CAP_L, 0), (T_L, T_C, CAP_C, CAP_L)):
                    cur = aff
                    curk = 'meaff'
                    for it in range(cap // 8):
                        sl = slice(o0 + it * 8, o0 + it * 8 + 8)
                        k.op('dve', lambda e, cur=cur, sl=sl: e.max(out=vals[:, sl], in_=cur[:, c0:c0 + T]), reads=[curk], writes=[('mevals', o0, it)])
                        k.op('dve', lambda e, cur=cur, sl=sl: e.max_index(out=idxu[:, sl], in_max=vals[:, sl], in_values=cur[:, c0:c0 + T]), reads=[curk, ('mevals', o0, it)], writes=[('meidx', o0, it)])
                        k.op('dve', lambda e, cur=cur, sl=sl: e.match_replace(out=wk_[:, c0:c0 + T], in_to_replace=vals[:, sl], in_values=cur[:, c0:c0 + T], imm_value=-1.0),
                             reads=[curk, ('mevals', o0, it)], writes=['mewk'])
                        cur = wk_
                        curk = 'mewk'
                if li + 1 < self.L and self.on('mods'):
                    self.phase_mods(li + 1, st2)
                vk = [('mevals', o0, it) for (o0, cap) in ((0, CAP_L), (CAP_L, CAP_C)) for it in range(cap // 8)]
                ik = [('meidx', o0, it) for (o0, cap) in ((0, CAP_L), (CAP_L, CAP_C)) for it in range(cap // 8)]
                k.op('dve', lambda e: e.tensor_copy(out=idxf[:], in_=idxu[:]), reads=ik, writes=['meidxf'])
                k.op('dve', lambda e: e.tensor_scalar(out=idxf[:, CAP_L:CAP_L + CAP_C], in0=idxf[:, CAP_L:CAP_L + CAP_C], scalar1=float(T_L), scalar2=None, op0=ALU.add),
                     reads=['meidxf'], writes=['meidxf'])
                for j, (a0, m) in enumerate(((0, 128), (128, 128), (256, 32))):
                    k.op('pe', lambda e, a0=a0, m=m: e.matmul(self.ps[0][0:m, 0:NE], lhsT=vals[:, a0:a0 + m], rhs=self.cfs[0:NE, K_ID:K_ID + NE], start=True, stop=True),
                         reads=vk + ['cfs'], writes=['ps0'])
                    k.op('dve', lambda e, j=j, m=m: e.tensor_copy(out=gcol[0:m, j, :], in_=self.ps[0][0:m, 0:NE]), reads=['ps0'], writes=['megcol'])
                    k.op('pe', lambda e, a0=a0, m=m: e.matmul(self.ps[1][0:m, 0:NE], lhsT=idxf[:, a0:a0 + m], rhs=self.cfs[0:NE, K_ID:K_ID + NE], start=True, stop=True),
                         reads=['meidxf', 'cfs'], writes=['ps1'])
                    k.op('dve', lambda e, j=j, m=m: e.tensor_copy(out=icol[0:m, j, :], in_=self.ps[1][0:m, 0:NE]), reads=['ps1'], writes=['meicol'])
            k.barrier()
            NW = 5
            wsl = [sb(st, nc, f"mew{i}", [128, 8192], BF16) for i in range(NW)]
            wcnt = [0]

            def wload(src_ap, view_shape):
                i = wcnt[0] % NW
                wcnt[0] += 1
                t = wsl[i]
                if view_shape[1] == 16:
                    v = t[:].rearrange("p (a b) -> p a b", a=16)
                else:
                    v = t[:].rearrange("p (a b) -> p a b", a=8)
                k.dma('pool', v, src_ap, writes=[f"mew{i}"])
                return v, f"mew{i}"

            g5 = sb(st, nc, "meg5", [128, 2, D], F32)
            for s in range(2):
                k.dma('sp', g5[:, s, :], self.MODS[li, s:s + 1, 5 * D:6 * D].broadcast_to([128, D]), reads=['MODS'], writes=['meg5'])
            xg = [sb(st, nc, f"mexg{i}", [128, 3, D], BF16) for i in range(2)]
            xeT = sb(st, nc, "mexeT", [128, 16, 288], BF16)
            sa = sb(st, nc, "mesa", [128, 288], F32)
            gT = sb(st, nc, "megT", [128, 8, 288], BF16)
            ysb = [sb(st, nc, f"mey{i}", [128, D], F32) for i in range(3)]
            ycnt = 0
            tiles = ((0, 128, 0, 0), (1, 128, 0, 0), (2, 32, 1, T_L))
            prev_sc = []
            def emit_gathers(ee):
                XG_ = xg[ee % 2]
                for (j, m, s, r0) in tiles:
                    k.dma('pool', XG_[0:m, j, :], self.XN2[:, :], reads=['XN2', 'meicol'], writes=[(f"mexg{ee % 2}", j)],
                          indirect=dict(out_offset=None, in_offset=bass.IndirectOffsetOnAxis(ap=icol[0:m, j, ee:ee + 1], axis=0)))

            def emit_scatter(ee, ys_):
                nonlocal prev_sc
                cur_sc = []
                for (j, m, s, r0) in tiles:
                    Y, yk = ys_[j]
                    cur_sc.append(k.dma('pool', self.XS[:, :], Y[0:m, :], reads=[(yk, a, b) for a in range(2) for b in range(2)] + ['meicol'],
                                        writes=[], deps=prev_sc,
                                        indirect=dict(out_offset=bass.IndirectOffsetOnAxis(ap=icol[0:m, j, ee:ee + 1], axis=0), in_offset=None, compute_op=ALU.add)))
                prev_sc = cur_sc

            pending = None
            emit_gathers(0)
            for e_ in range(NE):
                XG = xg[e_ % 2]
                xgk = f"mexg{e_ % 2}"
                if e_ + 1 < NE:
                    emit_gathers(e_ + 1)
                for kc in range(16):
                    pv = self.ps[kc % 2][:].bitcast(BF16)
                    pk = f"ps{kc % 2}"
                    for (j, m, s, r0) in tiles:
                        k.op('pe', lambda e, pv=pv, j=j, m=m, kc=kc: e.transpose(out=pv[:, j * 128:j * 128 + m], in_=XG[0:m, j, kc * 128:(kc + 1) * 128], identity=self.idb[0:m, 0:m]),
                             reads=[(xgk, j), 'idb'], writes=[pk])
                    k.op('act', lambda e, pv=pv, kc=kc: e.activation(out=xeT[:, kc, 0:256], in_=pv[:, 0:256], func=AF.Identity, scale=gs[:, 0, kc:kc + 1], bias=mc[:, 0, 0, kc:kc + 1]),
                         reads=[pk, 'gs3', 'mc3'], writes=[('mexeT', kc)])
                    k.op('act', lambda e, pv=pv, kc=kc: e.activation(out=xeT[:, kc, 256:288], in_=pv[:, 256:288], func=AF.Identity, scale=gs[:, 1, kc:kc + 1], bias=mc[:, 0, 1, kc:kc + 1]),
                         reads=[pk, 'gs3', 'mc3'], writes=[('mexeT', kc, 1)])
                xkeys = [('mexeT', kc) for kc in range(16)] + [('mexeT', kc, 1) for kc in range(16)]
                for fh in range(2):
                    wg, wgk = wload(self.w_eg[li, e_].rearrange("(kc p) f -> p kc f", p=128)[:, :, fh * 512:(fh + 1) * 512], (128, 16, 512))
                    wu, wuk = wload(self.w_eu[li, e_].rearrange("(kc p) f -> p kc f", p=128)[:, :, fh * 512:(fh + 1) * 512], (128, 16, 512))
                    for fc in range(4):
                        PA, PU = self.ps[2 + (fc % 2) * 2], self.ps[3 + (fc % 2) * 2]
                        pak, puk = f"ps{2 + (fc % 2) * 2}", f"ps{3 + (fc % 2) * 2}"
                        for kc in range(16):
                            k.op('pe', lambda e, kc=kc, fc=fc, PA=PA, wg=wg: e.matmul(PA[:, 0:288], lhsT=wg[:, kc, fc * 128:(fc + 1) * 128], rhs=xeT[:, kc, :], start=(kc == 0), stop=(kc == 15)),
                                 reads=([wgk] + xkeys) if kc == 0 else [], writes=[pak])
                        for kc in range(16):
                            k.op('pe', lambda e, kc=kc, fc=fc, PU=PU, wu=wu: e.matmul(PU[:, 0:288], lhsT=wu[:, kc, fc * 128:(fc + 1) * 128], rhs=xeT[:, kc, :], start=(kc == 0), stop=(kc == 15)),
                                 reads=([wuk] + xkeys) if kc == 0 else [], writes=[puk])
                        k.op('act', lambda e, PA=PA: e.activation(out=sa[:], in_=PA[:, 0:288], func=AF.Silu), reads=[pak], writes=['mesa'])
                        k.op('dve', lambda e, PU=PU, fh=fh, fc=fc: e.tensor_tensor(out=gT[:, fh * 4 + fc, :], in0=sa[:], in1=PU[:, 0:288], op=ALU.mult),
                             reads=['mesa', puk], writes=[('megT', fh * 4 + fc)])
                gkeys = [('megT', f) for f in range(8)]
                for dh in range(2):
                    wd, wdk = wload(self.w_ed[li, e_].rearrange("(fc p) d -> p fc d", p=128)[:, :, dh * 1024:(dh + 1) * 1024], (128, 8, 1024))
                    if dh == 0 and pending is not None:
                        emit_scatter(*pending)
                        pending = None
                    for (j, m, s, r0) in tiles:
                        if dh == 0:
                            Y = ysb[ycnt % 3]
                            yk = f"mey{ycnt % 3}"
                            ycnt += 1
                            if j == 0:
                                ys = []
                            ys.append((Y, yk))
                        else:
                            Y, yk = ys[j]
                        for dq in range(2):
                            P = self.ps[6 + dq]
                            pk = f"ps{6 + dq}"
                            d0 = dh * 1024 + dq * 512
                            for fc in range(8):
                                k.op('pe', lambda e, fc=fc, P=P, j=j, m=m, dq=dq, wd=wd: e.matmul(P[0:m, :], lhsT=gT[:, fc, j * 128:j * 128 + m], rhs=wd[:, fc, dq * 512:(dq + 1) * 512], start=(fc == 0), stop=(fc == 7)),
                                     reads=([wdk] + gkeys) if fc == 0 else [], writes=[pk])
                            k.op('dve', lambda e, P=P, m=m, j=j, s=s, d0=d0, Y=Y: e.scalar_tensor_tensor(out=Y[0:m, d0:d0 + 512], in0=P[0:m, :], scalar=gcol[0:m, j, e_:e_ + 1], in1=g5[0:m, s, d0:d0 + 512],
                                                                                                     op0=ALU.mult, op1=ALU.mult),
                                 reads=[pk, 'megcol', 'meg5'], writes=[(yk, dh, dq)])
                pending = (e_, ys)
            emit_scatter(*pending)

    def phase_final(self):
        nc, k = self.nc, self.k
        with ExitStack() as st:
            gb = sb(st, nc, "fngb", [128, D], F32)
            k.dma('sp', gb[:], self.fng.broadcast_to([128, D]), writes=['fngb'])
            xt = [sb(st, nc, f"fnx{i}", [128, D], F32) for i in range(2)]
            yt = [sb(st, nc, f"fny{i}", [128, D], F32) for i in range(2)]
            junk = sb(st, nc, "fnjunk", [128, D], BF16)
            ss = sb(st, nc, "fnss", [128, 16], F32)
            for ti in range(16):
                j = ti % 2
                k.dma('sp', xt[j][:], self.XS[ti * 128:(ti + 1) * 128, :], reads=['XS', 'XSacc'], writes=[f"fnx{j}"])
                k.op('act', lambda e, j=j, ti=ti: e.activation(out=junk[:], in_=xt[j][:], func=AF.Square, accum_out=ss[:, ti:ti + 1]), reads=[f"fnx{j}"], writes=['fnjunk', ('fnss', ti)])
                self.rstd_from_ss(ss[:, ti:ti + 1], ss[:, ti:ti + 1], D, [('fnss', ti)], [('fnss', ti)])
                k.op('dve', lambda e, j=j, ti=ti: e.scalar_tensor_tensor(out=yt[j][:], in0=xt[j][:], scalar=ss[:, ti:ti + 1], in1=gb[:], op0=ALU.mult, op1=ALU.mult),
                     reads=[f"fnx{j}", ('fnss', ti), 'fngb'], writes=[f"fny{j}"])
                k.dma('sp', self.out[ti * 128:(ti + 1) * 128, :], yt[j][:], reads=[f"fny{j}"], writes=['out'])


def make_consts():
    cf = np.zeros((128, NKF), np.float32)
    cf[:, K_ID:K_ID + 128] = np.eye(128, dtype=np.float32)
    cf[:, K_ONE:K_ONE + 128] = 1.0
    pm = np.zeros((128, 128), np.float32)
    for m in range(128):
        r = m % 32
        partner = m + 16 if r < 16 else m - 16
        pm[partner, m] = 1.0
    cf[:, K_PM:K_PM + 128] = pm
    j = np.arange(128)[:, None]
    i = np.arange(128)[None, :]
    same = (j // 64) == (i // 64)
    cf[:, K_UF:K_UF + 128] = (same & (j > i)).astype(np.float32)
    cf[:, K_UB:K_UB + 128] = (same & (j < i)).astype(np.float32)
    cf[:, K_MF:K_MF + 128] = (same & (j <= i)).astype(np.float32)
    cf[:, K_MB:K_MB + 128] = (same & (j >= i)).astype(np.float32)
    t = np.arange(T_L)
    row = (t // 64).astype(np.float32)
    col = (t % 64).astype(np.float32)
    inv_freq = (10000.0 ** (-np.arange(0, 32, 2, dtype=np.float32) / 32)).astype(np.float32)
    ang_r = row[:, None] * inv_freq
    ang_c = col[:, None] * inv_freq
    C = np.zeros((128, T_L), np.float32)
    S = np.zeros((128, T_L), np.float32)
    for p in range(128):
        r = p % 64
        ang = ang_r if r < 32 else ang_c
        f = r % 16
        first = (r % 32) < 16
        C[p] = np.cos(ang[:, f])
        S[p] = (-np.sin(ang[:, f])) if first else np.sin(ang[:, f])
    segm = np.ones((128, NT), np.float32)
    segm[:, ::64] = 0.0
    invc = np.zeros((4, NT), np.float32)
    for g, w in enumerate((2, 4, 8, 16)):
        for (c0, T) in ((0, T_L), (T_L, T_C)):
            tt = np.arange(T)
            lo = np.clip(tt - w // 2, 0, T)
            hi = np.clip(tt + w // 2, 0, T)
            invc[g, c0:c0 + T] = 1.0 / (hi - lo).astype(np.float32)
    return cf, C, S, segm, invc


def colize(v):
    return np.ascontiguousarray(v.reshape(-1, 128).T)


def make_cols(inp, layers):
    out = np.zeros((len(layers), 128, NCOL), np.float32)
    for a, l in enumerate(layers):
        c = out[a]
        c[:, C_N1G:C_N1G + 16] = colize(inp['norm1_g'][l])
        c[:, C_N2G:C_N2G + 16] = colize(inp['norm2_g'][l])
        c[:, C_PSC:C_PSC + 4] = colize(inp['pool_scale'][l])
        c[:, C_GBF:C_GBF + 4] = colize(inp['gla_gk_bias_f'][l])
        c[:, C_GBB:C_GBB + 4] = colize(inp['gla_gk_bias_b'][l])
        c[:, C_GNG:C_GNG + 1] = colize(inp['gla_norm_g'][l])
        c[:, C_CDB:C_CDB + 4] = colize(inp['conv_dw_b'][l])
        c[:, C_CLG:C_CLG + 4] = colize(inp['conv_ln_g'][l])
        c[:, C_CLB:C_CLB + 4] = colize(inp['conv_ln_b'][l])
        c[:, C_CPB:C_CPB + 4] = colize(inp['conv_pw_b'][l])
        c[:, C_SLG:C_SLG + 1] = colize(inp['diff_subln_g'][l])
        lam_init = 0.8 - 0.6 * math.exp(-0.3 * l)
        c[:, C_LAMI] = lam_init
        c[:, C_OML] = 1.0 - lam_init
        dw = inp['conv_dw'][l]
        c[:, C_DW:C_DW + 124] = dw.reshape(31, 4, 128).transpose(2, 1, 0).reshape(128, 124)
        for j, nm in enumerate(('diff_lq1', 'diff_lk1', 'diff_lq2', 'diff_lk2')):
            c[:, C_LQ + 64 * j:C_LQ + 64 * (j + 1)] = inp[nm][l][None, :]
    return out


_CACHE = {}


def build_prog(nl, with_final, debug=False, phases=None):
    key = (nl, with_final, debug, tuple(phases) if phases else None)
    if key in _CACHE:
        return _CACHE[key]
    nc = bass.Bass("TRN2", target_bir_lowering=False)
    p = Prog(nc, list(range(nl)), with_final, debug=debug, phases=phases)
    p.build()
    _CACHE[key] = (nc, p)
    return nc, p


def layer_inputs(inp, layers, consts):
    cf, C, S, segm, invc = consts
    ls = list(layers)
    sl = lambda a: np.ascontiguousarray(a[ls])
    up_f = np.concatenate([inp['gla_gk_up_f'][ls], inp['gla_gk_bias_f'][ls][:, None, :]], axis=1)
    up_b = np.concatenate([inp['gla_gk_up_b'][ls], inp['gla_gk_bias_b'][ls][:, None, :]], axis=1)
    return dict(
        cf=cf, ropeC=C, ropeS=S, segm=segm, invc=invc, cols=make_cols(inp, ls),
        b_ada=np.ascontiguousarray(np.repeat(inp['b_ada'][ls][:, None, :], 2, axis=1)),
        w_ada=sl(inp['w_ada']), w_in=sl(inp['w_in']), pool_w=sl(inp['pool_w']),
        up_f=np.ascontiguousarray(up_f), up_b=np.ascontiguousarray(up_b),
        conv_pw=sl(inp['conv_pw']), w_out=sl(inp['w_out']), w_router=sl(inp['w_router']),
        w_eg=sl(inp['w_exp_gate']), w_eu=sl(inp['w_exp_up']), w_ed=sl(inp['w_exp_down']),
        fng=np.ascontiguousarray(inp['final_norm_g'][None, :]),
    )


def kernel(**inputs):
    inp = {k_: np.asarray(v) for k_, v in inputs.items()}
    consts = make_consts()
    B = inp['x'].shape[0]
    xs = [np.ascontiguousarray(np.concatenate([inp['x'][b], inp['ctx'][b]], axis=0)) for b in range(B)]
    cT = []
    for b in range(B):
        cc = np.stack([colize(inp['c'][b]), colize(inp['c_ctx'])], axis=-1)
        cT.append(np.ascontiguousarray(cc.astype(np.float32)))
    FUSED = True
    if FUSED:
        nc, p = build_prog(DEPTH, True)
        shared = layer_inputs(inp, range(DEPTH), consts)
        in_maps = [dict(shared, xs_in=xs[b], cT=cT[b]) for b in range(B)]
        res = run_bass_kernel_spmd(nc, in_maps, core_ids=list(range(B)))
        return np.stack([res.results[b]["out"] for b in range(B)], axis=0)
    out = None
    for l in range(DEPTH):
        last = l == DEPTH - 1
        nc, p = build_prog(1, last)
        shared = layer_inputs(inp, [l], consts)
        in_maps = [dict(shared, xs_in=xs[b], cT=cT[b]) for b in range(B)]
        res = run_bass_kernel_spmd(nc, in_maps, core_ids=list(range(B)))
        if last:
            out = np.stack([res.results[b]["out"] for b in range(B)], axis=0)
        else:
            xs = [res.results[b]["XS"] for b in range(B)]
    return out
```
